# Optimizing a Trainium2 kernel written in Bass

```python
import math
import jax, jax.numpy as jnp
from jax import lax
import numpy as np

D_MODEL = 2048
BATCH = 4
SEQ = 4096
DEPTH = 1
DEC_BATCH = 32
DEC_SEQ = 16
PAST_LEN = 4096

CHUNK = 64
SB_HEADS = 8
HEAD_DIM = 128
ATT_DIM = SB_HEADS * HEAD_DIM
CONV_DIM = 1024
CONV_WIDTH = 3
Q_BLOCK = 128
PEER_HEADS = 8
PEER_QDIM = 256
PEER_HALF = PEER_QDIM // 2
N_KEYS = 128
N_EXPERTS = N_KEYS * N_KEYS
PEER_TOPK = 16
PEER_BLOCK = 256
N_MOD = 6
IN_COLS = 3 * ATT_DIM + 3 * CONV_DIM + 2 * D_MODEL
RMS_EPS = 1e-6

kernel_name = "stickbreak_shortconv_peer_stream_step"


def _rmsnorm(x, g):
    xf = x.astype(jnp.float32)
    r = lax.rsqrt(jnp.mean(xf * xf, axis=-1, keepdims=True) + RMS_EPS)
    return (xf * r).astype(x.dtype) * g


def _stick_breaking(q, k, v, q_pos, k_pos):
    z = jnp.einsum('bqhd,bkhd->bhqk', q, k).astype(jnp.float32) * (HEAD_DIM ** -0.5)
    mask = k_pos[None, :] < q_pos[:, None]
    log_keep = jnp.where(mask, jax.nn.log_sigmoid(-z), 0.0)
    later = lax.cumsum(log_keep, axis=3, reverse=True) - log_keep
    a = jnp.where(mask, jnp.exp(jax.nn.log_sigmoid(z) + later), 0.0)
    return jnp.einsum('bhqk,bkhd->bqhd', a.astype(v.dtype), v)


def _token_mixer(xn, w_in, conv_w, w_attn_proj, w_conv_out, w_o, past_k, past_v, conv_state):
    b, t, _ = xn.shape
    proj = xn @ w_in
    offs = [ATT_DIM, 2 * ATT_DIM, 3 * ATT_DIM,
            3 * ATT_DIM + CONV_DIM, 3 * ATT_DIM + 2 * CONV_DIM, 3 * ATT_DIM + 3 * CONV_DIM,
            3 * ATT_DIM + 3 * CONV_DIM + D_MODEL]
    q, k, v, hc, gb, gc, pre_ga, pre_gc = jnp.split(proj, offs, axis=-1)
    q = q.reshape(b, t, SB_HEADS, HEAD_DIM)
    k = k.reshape(b, t, SB_HEADS, HEAD_DIM)
    v = v.reshape(b, t, SB_HEADS, HEAD_DIM)
    if past_k is None:
        nb = t // Q_BLOCK
        k_pos = jnp.arange(t, dtype=jnp.int32)
        qb = q.reshape(b, nb, Q_BLOCK, SB_HEADS, HEAD_DIM).transpose(1, 0, 2, 3, 4)
        starts = jnp.arange(nb, dtype=jnp.int32) * Q_BLOCK
        o = lax.map(lambda a: _stick_breaking(a[0], k, v, a[1] + jnp.arange(Q_BLOCK, dtype=jnp.int32), k_pos),
                    (qb, starts))
        o = o.transpose(1, 0, 2, 3, 4).reshape(b, t, ATT_DIM)
        u_prev = jnp.zeros((b, CONV_WIDTH - 1, CONV_DIM), xn.dtype)
    else:
        p = past_k.shape[1]
        k_all = jnp.concatenate([past_k.astype(k.dtype), k], axis=1)
        v_all = jnp.concatenate([past_v.astype(v.dtype), v], axis=1)
        q_pos = p + jnp.arange(t, dtype=jnp.int32)
        k_pos = jnp.arange(p + t, dtype=jnp.int32)
        o = _stick_breaking(q, k_all, v_all, q_pos, k_pos).reshape(b, t, ATT_DIM)
        u_prev = conv_state.astype(xn.dtype)
    u = gc * hc
    upad = jnp.concatenate([u_prev, u], axis=1)
    conv = conv_w[0] * upad[:, 0:t]
    for i in range(1, CONV_WIDTH):
        conv = conv + conv_w[i] * upad[:, i:i + t]
    y_conv = (gb * conv) @ w_conv_out
    y_att = o @ w_attn_proj
    merged = jax.nn.sigmoid(pre_ga) * y_att + jax.nn.sigmoid(pre_gc) * y_conv
    return merged @ w_o, k, v, upad[:, -(CONV_WIDTH - 1):]


def _peer(h, w_query, sub_keys, expert_u, expert_v):
    b, t, d = h.shape
    n = b * t
    x2 = h.reshape(n, d)
    q = (x2 @ w_query).reshape(n, PEER_HEADS, 2, PEER_HALF)
    s = jnp.einsum('nhpd,hpkd->nhpk', q, sub_keys).astype(jnp.float32)
    s1, i1 = lax.top_k(s[:, :, 0], PEER_TOPK)
    s2, i2 = lax.top_k(s[:, :, 1], PEER_TOPK)
    cand = (s1[..., :, None] + s2[..., None, :]).reshape(n, PEER_HEADS, PEER_TOPK * PEER_TOPK)
    cidx = (i1[..., :, None] * N_KEYS + i2[..., None, :]).reshape(n, PEER_HEADS, PEER_TOPK * PEER_TOPK)
    top, pos = lax.top_k(cand, PEER_TOPK)
    eidx = jnp.take_along_axis(cidx, pos, axis=-1).reshape(n, PEER_HEADS * PEER_TOPK)
    gate = jax.nn.softmax(top, axis=-1).reshape(n, PEER_HEADS * PEER_TOPK).astype(h.dtype)
    nb = -(-n // PEER_BLOCK)
    pad = nb * PEER_BLOCK - n
    xb = jnp.pad(x2, ((0, pad), (0, 0))).reshape(nb, PEER_BLOCK, d)
    eb = jnp.pad(eidx, ((0, pad), (0, 0))).reshape(nb, PEER_BLOCK, PEER_HEADS * PEER_TOPK)
    gbk = jnp.pad(gate, ((0, pad), (0, 0))).reshape(nb, PEER_BLOCK, PEER_HEADS * PEER_TOPK)

    def body(a):
        xx, ee, gg = a
        pre = jnp.einsum('nd,ned->ne', xx, expert_u[ee])
        w = gg * jax.nn.gelu(pre, approximate=False)
        return jnp.einsum('ne,ned->nd', w, expert_v[ee])

    out = lax.map(body, (xb, eb, gbk)).reshape(nb * PEER_BLOCK, d)[:n]
    return out.reshape(b, t, d)


def _layer(x, c, past_k, past_v, conv_state, norm1_g, norm2_g, w_ada, b_ada, w_in, conv_w,
           w_attn_proj, w_conv_out, w_o, w_query, sub_keys, expert_u, expert_v):
    mod = jax.nn.silu(c) @ w_ada + b_ada
    sh1, sc1, g1, sh2, sc2, g2 = jnp.split(mod[:, None, :], N_MOD, axis=-1)
    h = _rmsnorm(x, norm1_g) * (1 + sc1) + sh1
    mix, k, v, st = _token_mixer(h, w_in, conv_w, w_attn_proj, w_conv_out, w_o, past_k, past_v, conv_state)
    x = x + g1 * mix
    h = _rmsnorm(x, norm2_g) * (1 + sc2) + sh2
    x = x + g2 * _peer(h, w_query, sub_keys, expert_u, expert_v)
    return x, k, v, st


def setup_inputs(seed: int = 0) -> dict:
    key = jax.random.key(seed)
    ks = jax.random.split(key, 24)
    f32 = jnp.float32
    nrm = lambda k, shape, s: jax.random.normal(k, shape, f32) * s
    return {
        "x_prompt": nrm(ks[0], (BATCH, SEQ, D_MODEL), 1.0),
        "x_sample": nrm(ks[1], (DEC_BATCH, DEC_SEQ, D_MODEL), 1.0),
        "c_prompt": nrm(ks[2], (BATCH, D_MODEL), 1.0),
        "c_sample": nrm(ks[3], (DEC_BATCH, D_MODEL), 1.0),
        "cache_k": nrm(ks[4], (DEPTH, DEC_BATCH, PAST_LEN, SB_HEADS, HEAD_DIM), 1.0),
        "cache_v": nrm(ks[5], (DEPTH, DEC_BATCH, PAST_LEN, SB_HEADS, HEAD_DIM), 1.0),
        "state_conv": nrm(ks[6], (DEPTH, DEC_BATCH, CONV_WIDTH - 1, CONV_DIM), 1.0),
        "norm1_g": 1.0 + nrm(ks[7], (DEPTH, D_MODEL), 0.02),
        "norm2_g": 1.0 + nrm(ks[8], (DEPTH, D_MODEL), 0.02),
        "w_ada": nrm(ks[9], (DEPTH, D_MODEL, N_MOD * D_MODEL), 0.5 * D_MODEL ** -0.5),
        "b_ada": nrm(ks[10], (DEPTH, N_MOD * D_MODEL), 0.01),
        "w_in": nrm(ks[11], (DEPTH, D_MODEL, IN_COLS), D_MODEL ** -0.5),
        "conv_w": nrm(ks[12], (DEPTH, CONV_WIDTH, CONV_DIM), CONV_WIDTH ** -0.5),
        "w_attn_proj": nrm(ks[13], (DEPTH, ATT_DIM, D_MODEL), ATT_DIM ** -0.5),
        "w_conv_out": nrm(ks[14], (DEPTH, CONV_DIM, D_MODEL), CONV_DIM ** -0.5),
        "w_o": nrm(ks[15], (DEPTH, D_MODEL, D_MODEL), D_MODEL ** -0.5),
        "w_query": nrm(ks[16], (DEPTH, D_MODEL, PEER_HEADS * PEER_QDIM), D_MODEL ** -0.5),
        "sub_keys": nrm(ks[17], (DEPTH, PEER_HEADS, 2, N_KEYS, PEER_HALF), PEER_HALF ** -0.5),
        "expert_u": nrm(ks[18], (DEPTH, N_EXPERTS, D_MODEL), D_MODEL ** -0.5),
        "expert_v": nrm(ks[19], (DEPTH, N_EXPERTS, D_MODEL), PEER_HEADS ** -0.5),
        "final_norm_g": 1.0 + nrm(ks[20], (D_MODEL,), 0.02),
    }


def reference(x_prompt, x_sample, c_prompt, c_sample, cache_k, cache_v, state_conv, norm1_g, norm2_g,
              w_ada, b_ada, w_in, conv_w, w_attn_proj, w_conv_out, w_o, w_query, sub_keys,
              expert_u, expert_v, final_norm_g):
    xp, xs = x_prompt, x_sample
    kp_l, vp_l, sp_l, ks_l, vs_l, ss_l = [], [], [], [], [], []
    for l in range(DEPTH):
        lw = (norm1_g[l], norm2_g[l], w_ada[l], b_ada[l], w_in[l], conv_w[l], w_attn_proj[l],
              w_conv_out[l], w_o[l], w_query[l], sub_keys[l], expert_u[l], expert_v[l])
        xp, kp, vp, sp = _layer(xp, c_prompt, None, None, None, *lw)
        xs, kk, vv, ss = _layer(xs, c_sample, cache_k[l], cache_v[l], state_conv[l], *lw)
        kp_l.append(kp); vp_l.append(vp); sp_l.append(sp)
        ks_l.append(kk); vs_l.append(vv); ss_l.append(ss)
    y_prompt = _rmsnorm(xp, final_norm_g)
    y_sample = _rmsnorm(xs, final_norm_g)
    return (y_prompt, y_sample, jnp.stack(kp_l), jnp.stack(vp_l), jnp.stack(sp_l),
            jnp.stack(ks_l), jnp.stack(vs_l), jnp.stack(ss_l))
```

```python
from contextlib import ExitStack
import numpy as np
import concourse.bass as bass
import concourse.mybir as mybir
from concourse.bass_utils import run_bass_kernel_spmd

F32 = mybir.dt.float32
BF16 = mybir.dt.bfloat16
U32 = mybir.dt.uint32
I32 = mybir.dt.int32
AF = mybir.ActivationFunctionType
ALU = mybir.AluOpType
AX = mybir.AxisListType

D = 2048
NCTX = 2048
NOWN = 2112
NTOK = NCTX + NOWN
NSEQ = 5
INC = 10240
NEXP = 16384
SEM_LIMIT = 20000
NEG = -1.0e30


class Buf:
    __slots__ = ("name", "w", "r", "excl")

    def __init__(self, name="", excl=False):
        self.name = name
        self.w = None
        self.r = {}
        self.excl = excl


class Eng:
    def __init__(self, K, name, h, same_sync):
        self.K = K
        self.name = name
        self.h = h
        self.same_sync = same_sync
        self.sem = K.new_sem(name)
        self.count = 0
        self.known = {}
        self.nsem = 0
        self.dpool = []
        self.dnext = 0

    def new_counter(self):
        self.nsem += 1
        self.sem = self.K.new_sem(f"{self.name}{self.nsem}")
        self.count = 0


class Kern:
    def __init__(self, nc, ndma_sems=10):
        self.nc = nc
        self._nsem = 0
        self.pe = Eng(self, "pe", nc.tensor, False)
        self.act = Eng(self, "act", nc.scalar, True)
        self.dve = Eng(self, "dve", nc.vector, True)
        self.pool = Eng(self, "pool", nc.gpsimd, True)
        self.sp = Eng(self, "sp", nc.sync, False)
        self.engs = [self.pe, self.act, self.dve, self.pool, self.sp]
        for e in (self.sp, self.act, self.pool):
            for i in range(ndma_sems):
                e.dpool.append([self.new_sem(f"d{e.name}{i}"), 0])
        self.out_events = []

    def new_sem(self, name):
        self._nsem += 1
        return self.nc.alloc_semaphore(f"s_{name}_{self._nsem}")

    def _wait(self, E, ev):
        if ev is None:
            return
        sem, val = ev
        if E.known.get(id(sem), (None, 0))[1] >= val:
            return
        E.h.wait_ge(sem, val)
        E.known[id(sem)] = (sem, val)

    def _deps(self, E, reads, writes):
        evs = {}

        def add(ev):
            if ev is None:
                return
            k = id(ev[0])
            if k not in evs or evs[k][1] < ev[1]:
                evs[k] = ev
        for b in reads:
            add(b.w)
        for b in writes:
            add(b.w)
            for ev in b.r.values():
                add(ev)
        for ev in evs.values():
            if (not E.same_sync) and ev[0] is E.sem:
                continue
            self._wait(E, ev)

    def _record(self, ev, reads, writes):
        k = id(ev[0])
        for b in reads:
            if k not in b.r or b.r[k][1] < ev[1]:
                b.r[k] = ev
        for b in writes:
            b.w = ev
            b.r = {}

    def op(self, E, fn, reads=(), writes=(), signal=True):
        ex = [b for b in reads if b.excl]
        if ex:
            writes = list(writes) + ex
        self._deps(E, reads, writes)
        ins = fn(E.h)
        if signal:
            if E.count >= SEM_LIMIT:
                E.new_counter()
            E.count += 1
            ins.then_inc(E.sem, 1)
            ev = (E.sem, E.count)
            self._record(ev, reads, writes)
            return ev
        return None

    def dma(self, E, out, in_, reads=(), writes=(), is_output=False, **kw):
        self._deps(E, reads, writes)
        slot = E.dpool[E.dnext % len(E.dpool)]
        E.dnext += 1
        if slot[1] > 0:
            self._wait(E, (slot[0], slot[1]))
        if slot[1] >= SEM_LIMIT:
            slot[0] = self.new_sem("dx")
            slot[1] = 0
        ins = E.h.dma_start(out=out, in_=in_, **kw)
        slot[1] += 16
        ins.then_inc(slot[0], 16)
        ev = (slot[0], slot[1])
        self._record(ev, reads, writes)
        if is_output:
            self.out_events.append(ev)
        return ev

    def barrier(self):
        evs = []
        for e in self.engs:
            if e.count > 0:
                evs.append((e.sem, e.count))
        for e in (self.sp, self.act, self.pool):
            for slot in e.dpool:
                if slot[1] > 0:
                    evs.append((slot[0], slot[1]))
        for E in self.engs:
            for ev in evs:
                if ev[0] is E.sem:
                    continue
                self._wait(E, ev)

    def finish(self):
        self.barrier()


LAST_INPUT_NAMES = set()


def build(debug=False, stop_after=99):
    nc = bass.Bass("TRN2", target_bir_lowering=False)
    K = Kern(nc)
    PE, ACT, DVE, POOL, SP = K.pe, K.act, K.dve, K.pool, K.sp

    LAST_INPUT_NAMES.clear()

    def din(name, shape, dt=F32):
        LAST_INPUT_NAMES.add(name)
        return nc.dram_tensor(name, list(shape), dt, kind="ExternalInput").ap()

    def dout(name, shape, dt=F32):
        return nc.dram_tensor(name, list(shape), dt, kind="ExternalOutput").ap()

    def dscr(name, shape, dt):
        kind = "ExternalOutput" if (debug and name in debug) else "Internal"
        return nc.dram_tensor(name, list(shape), dt, kind=kind).ap()

    xall = din("xall", [NTOK, D])
    cvec = din("cvec", [NSEQ, D])
    flags = din("flags", [128, 2])
    cache_k = din("cache_k", [4, 4096, 1024]) if stop_after > 2.5 else None
    cache_v = din("cache_v", [4, 4096, 1024]) if stop_after > 2.5 else None
    state_conv = din("state_conv", [4, 2, 1024])
    norm1_g = din("norm1_g", [D])
    norm2_g = din("norm2_g", [D])
    final_g = din("final_g", [D])
    w_ada = din("w_ada", [D, 6 * D])
    b_ada = din("b_ada", [6 * D])
    w_in = din("w_in", [D, INC])
    conv_w = din("conv_w", [3, 1024])
    w_ap = din("w_attn_proj", [1024, D]) if stop_after >= 4 else None
    w_co = din("w_conv_out", [1024, D]) if stop_after >= 4 else None
    w_o = din("w_o", [D, D]) if stop_after >= 4 else None
    w_q = din("w_query", [D, D]) if stop_after >= 5 else None
    sub_keys = din("sub_keys", [16, 128, 128]) if stop_after >= 5 else None
    exp_u = din("expert_u", [NEXP, D]) if stop_after >= 6 else None
    exp_v = din("expert_v", [NEXP, D]) if stop_after >= 7 else None
    y_out = dout("y", [NOWN, D])
    nk_out = dout("nk", [NOWN, 1024])
    nv_out = dout("nv", [NOWN, 1024])
    nconv_out = dout("nconv", [NSEQ, 2, 1024])
    gS_d = dscr("gS_d", [2, NSEQ, D], F32)
    hT_d = dscr("hT_d", [16, 128, NTOK], BF16)
    KT_d = dscr("KT_d", [8, 128, NTOK], BF16)
    V_d = dscr("V_d", [NTOK, 1024], BF16)
    QT_d = dscr("QT_d", [8, 128, NOWN], BF16)
    GC_d = dscr("GC_d", [8, 128, NOWN], BF16)
    SG_d = dscr("SG_d", [2, 16, 128, NOWN], BF16)
    OT_d = dscr("OT_d", [8, 128, NOWN], BF16)
    X1_d = dscr("X1_d", [NOWN, D], F32)
    H2_d = dscr("H2_d", [16, 128, NOWN], BF16)
    MG_d = dscr("MG_d", [16, 128, NOWN], BF16)
    S_d = dscr("S_d", [17, 128, 2048], F32)
    Gs_d = dscr("Gs_d", [17, 128, 128, 128], BF16)
    Ws_d = dscr("Ws_d", [128, 128, NOWN], BF16)
    dbufs = {n: Buf(n) for n in ["gS", "hT", "KT", "V", "QT", "GC", "SG", "OT", "X1", "H2", "S", "Gs", "Ws", "MG"]}

    psb = [nc.alloc_psum_tensor(f"psb{i}", [128, 512], F32) for i in range(8)]
    pbuf = [Buf(f"ps{i}", excl=True) for i in range(8)]

    def sbp(name, shape, dt=F32):
        return nc.alloc_sbuf_tensor(name, list(shape), dt)

    ident = sbp("ident", [128, 128], BF16)
    nti = sbp("nti", [128, 128], BF16)
    neg1 = sbp("neg1", [128, 128], BF16)
    maskd = sbp("maskd", [128, 4, 512], BF16)
    masks = sbp("masks", [16, 8, 16], BF16)
    flg = sbp("flg", [128, 2], F32)
    zero1 = sbp("zero1", [128, 1], F32)
    eps1 = sbp("eps1", [128, 1], F32)
    iota16 = sbp("iota16", [128, 16], F32)
    A1 = sbp("A1", [128, 16, NSEQ], F32)
    B1 = sbp("B1", [128, 16, NSEQ], F32)
    A2 = sbp("A2", [128, 16, NSEQ], F32)
    B2 = sbp("B2", [128, 16, NSEQ], F32)
    fgT = sbp("fgT", [128, 16], F32)
    cw = sbp("cw", [128, 3, 8], F32)
    uprev = sbp("uprev", [128, 8, 2], F32)
    cst = Buf("consts")
    bmod = Buf("mod")
    buprev = Buf("uprev")

    nc_allow = nc.allow_non_contiguous_dma(reason="small strided parameter loads")
    nc_allow.__enter__()

    tmpf = sbp("tmpf", [128, 512], F32)
    K.op(POOL, lambda e: e.memset(tmpf[:, 0:128], 0.0), writes=[cst])
    K.op(POOL, lambda e: e.affine_select(out=tmpf[:, 0:128], in_=tmpf[:, 0:128], pattern=[[-1, 128]],
                                         compare_op=ALU.not_equal, fill=1.0, base=0, channel_multiplier=1),
         reads=[cst], writes=[cst])
    K.op(POOL, lambda e: e.tensor_copy(out=ident[:], in_=tmpf[:, 0:128]), reads=[cst], writes=[cst])
    K.op(POOL, lambda e: e.memset(tmpf[:, 0:128], -1.0), reads=[cst], writes=[cst])
    K.op(POOL, lambda e: e.tensor_copy(out=neg1[:], in_=tmpf[:, 0:128]), reads=[cst], writes=[cst])
    K.op(POOL, lambda e: e.affine_select(out=tmpf[:, 0:128], in_=tmpf[:, 0:128], pattern=[[-1, 128]],
                                         compare_op=ALU.is_ge, fill=0.0, base=0, channel_multiplier=1),
         reads=[cst], writes=[cst])
    K.op(POOL, lambda e: e.tensor_copy(out=nti[:], in_=tmpf[:, 0:128]), reads=[cst], writes=[cst])
    for m in range(4):
        K.op(POOL, lambda e: e.memset(tmpf[:], 1.0), reads=[cst], writes=[cst])
        K.op(POOL, lambda e, m=m: e.affine_select(out=tmpf[:], in_=tmpf[:], pattern=[[1, 512]],
                                                  compare_op=ALU.is_gt, fill=0.0, base=-128 * m,
                                                  channel_multiplier=-1), reads=[cst], writes=[cst])
        K.op(POOL, lambda e, m=m: e.tensor_copy(out=maskd[:, m, :], in_=tmpf[:]), reads=[cst], writes=[cst])
    K.op(POOL, lambda e: e.memset(tmpf[0:16, 0:128], 1.0), reads=[cst], writes=[cst])
    K.op(POOL, lambda e: e.affine_select(out=tmpf[0:16, 0:128].rearrange("p (h i) -> p h i", h=8),
                                         in_=tmpf[0:16, 0:128].rearrange("p (h i) -> p h i", h=8),
                                         pattern=[[0, 8], [1, 16]], compare_op=ALU.is_gt, fill=0.0, base=0,
                                         channel_multiplier=-1), reads=[cst], writes=[cst])
    K.op(POOL, lambda e: e.tensor_copy(out=masks[:], in_=tmpf[0:16, 0:128].rearrange("p (h i) -> p h i", h=8)),
         reads=[cst], writes=[cst])
    K.op(POOL, lambda e: e.memset(zero1[:], 0.0), reads=[cst], writes=[cst])
    K.op(POOL, lambda e: e.memset(eps1[:], 1e-6), reads=[cst], writes=[cst])
    K.op(POOL, lambda e: e.iota(iota16[:], pattern=[[1, 16]], base=0, channel_multiplier=0,
                                allow_small_or_imprecise_dtypes=True), reads=[cst], writes=[cst])
    K.dma(SP, flg[:], flags[:, :], writes=[cst])
    K.dma(SP, fgT[:], final_g.rearrange("(c p) -> p c", p=128), writes=[cst])
    for r in range(3):
        K.dma(SP, cw[:, r, :], conv_w[r].rearrange("(j p) -> p j", p=128), writes=[cst])

    with ExitStack() as st:
        def sb(name, shape, dt=F32):
            return st.enter_context(nc.sbuf_tensor(name, list(shape), dt))
        cT = sb("cT", [128, 16, NSEQ])
        bcT = Buf()
        bA = sb("bA", [128, 96])
        n1g = sb("n1g", [128, 16])
        n2g = sb("n2g", [128, 16])
        modT = sb("modT", [128, 6, 16, NSEQ])
        wbuf = [sb(f"wada{i}", [128, 16, 512]) for i in range(2)]
        bwbuf = [Buf(), Buf()]
        gtm = sb("gtm", [NSEQ, 2, D])
        brow = sb("brow", [NSEQ, 2, D])
        bg = Buf()
        for s_ in range(NSEQ):
            K.dma(SP, cT[:, :, s_], cvec[s_].rearrange("(c p) -> p c", p=128), writes=[bcT])
        K.dma(SP, bA[:], b_ada.rearrange("(b p) -> p b", p=128), writes=[bmod])
        K.dma(SP, n1g[:], norm1_g.rearrange("(c p) -> p c", p=128), writes=[bmod])
        K.dma(SP, n2g[:], norm2_g.rearrange("(c p) -> p c", p=128), writes=[bmod])
        for wi, which in enumerate((2, 5)):
            K.dma(SP, brow[:, wi, :], b_ada[which * D:(which + 1) * D].unsqueeze(0).to_broadcast([NSEQ, D]),
                  writes=[bg])
        K.op(ACT, lambda e: e.activation(out=cT[:], in_=cT[:], func=AF.Silu), reads=[bcT], writes=[bcT])
        cTb = sb("cTb", [128, 16, NSEQ], BF16)
        K.op(DVE, lambda e: e.tensor_copy(out=cTb[:], in_=cT[:]), reads=[bcT], writes=[bcT])
        wadab = [sb(f"wadab{i}", [128, 16, 512], BF16) for i in range(2)]
        bwadab = [Buf(), Buf()]
        gi = 0
        for which in range(6):
            for q4 in range(4):
                col0 = which * D + q4 * 512
                wb, bw = wbuf[gi % 2], bwbuf[gi % 2]
                K.dma(SP if gi % 2 == 0 else POOL, wb[:], w_ada[:, col0:col0 + 512].rearrange("(c p) n -> p c n", p=128),
                      writes=[bw])
                pi = gi % 2
                if which in (2, 5):
                    wi = 0 if which == 2 else 1
                    for c in range(16):
                        K.op(PE, lambda e, c=c: e.matmul(psb[pi][0:NSEQ, :], lhsT=cT[:, c, :], rhs=wb[:, c, :],
                                                         start=(c == 0), stop=(c == 15)),
                             reads=[bcT, bw], writes=[pbuf[pi]], signal=(c == 15))
                    K.op(DVE, lambda e: e.tensor_tensor(out=gtm[:, wi, q4 * 512:(q4 + 1) * 512], in0=psb[pi][0:NSEQ, :],
                                                        in1=brow[:, wi, q4 * 512:(q4 + 1) * 512], op=ALU.add),
                         reads=[pbuf[pi], bg], writes=[bg])
                else:
                    wbb, bwbb = wadab[gi % 2], bwadab[gi % 2]
                    K.op(DVE if gi % 2 == 0 else ACT,
                         (lambda e: e.tensor_copy(out=wbb[:], in_=wb[:])) if gi % 2 == 0 else
                         (lambda e: e.activation(out=wbb[:], in_=wb[:], func=AF.Copy)), reads=[bw], writes=[bwbb])
                    for j in range(4):
                        for c in range(16):
                            K.op(PE, lambda e, c=c, j=j: e.matmul(psb[pi][:, j * NSEQ:(j + 1) * NSEQ],
                                                                  lhsT=wbb[:, c, j * 128:(j + 1) * 128], rhs=cTb[:, c, :],
                                                                  start=(c == 0), stop=(c == 15)),
                                 reads=[bcT, bwbb], writes=[pbuf[pi]], signal=(c == 15 and j == 3))
                    blk0 = which * 16 + q4 * 4
                    K.op(DVE, lambda e: e.tensor_tensor(
                        out=modT[:, which, q4 * 4:(q4 + 1) * 4, :],
                        in0=psb[pi][:, 0:4 * NSEQ].rearrange("p (j s) -> p j s", j=4),
                        in1=bA[:, blk0:blk0 + 4].unsqueeze(2).to_broadcast([128, 4, NSEQ]), op=ALU.add),
                         reads=[pbuf[pi], bmod], writes=[bmod])
                gi += 1
        for (Aa, Bb, gg, isc, ish) in ((A1, B1, n1g, 1, 0), (A2, B2, n2g, 4, 3)):
            K.op(DVE, lambda e, Aa=Aa, isc=isc: e.tensor_scalar(out=Aa[:], in0=modT[:, isc, :, :], scalar1=1.0, scalar2=None,
                                                                op0=ALU.add), reads=[bmod], writes=[bmod])
            K.op(DVE, lambda e, Aa=Aa, gg=gg: e.tensor_tensor(out=Aa[:], in0=Aa[:],
                                                              in1=gg[:].unsqueeze(2).to_broadcast([128, 16, NSEQ]),
                                                              op=ALU.mult), reads=[bmod], writes=[bmod])
            K.op(DVE, lambda e, Bb=Bb, ish=ish: e.tensor_copy(out=Bb[:], in_=modT[:, ish, :, :]), reads=[bmod],
                 writes=[bmod])
        K.dma(SP, gS_d.rearrange("w s d -> s w d"), gtm[:], reads=[bg], writes=[dbufs["gS"]])
        K.barrier()
    if stop_after <= 0:
        K.finish()
        return nc

    def seq_groups(tb):
        if tb < 32:
            return [(0, 0, 128)]
        return [(1 + i, 16 * i, 16) for i in range(4)]

    class NormCtx:
        def __init__(self, st, tag):
            self.junk = st.enter_context(nc.sbuf_tensor(f"junk{tag}", [128, D], BF16))
            self.xn = [st.enter_context(nc.sbuf_tensor(f"xn{tag}{i}", [128, D], BF16)) for i in range(2)]
            self.bxn = [Buf(), Buf()]
            self.ss = [st.enter_context(nc.sbuf_tensor(f"ss{tag}{i}", [128, 2], F32)) for i in range(2)]
            self.bss = [Buf(), Buf()]
            self.bjunk = Buf()
            self.i = 0

    def emit_norm_T(ctx, xt, bxt, tp, groups, Aa, Bb, hT_tile, bhT, tcol0, pbanks):
        st_ = emit_norm_stats(ctx, xt, bxt, tp)
        emit_norm_tr(ctx, st_, tp, groups, Aa, Bb, hT_tile, bhT, tcol0, pbanks)

    def emit_norm_stats(ctx, xt, bxt, tp):
        i = ctx.i
        ctx.i += 1
        xn, bxn, ss, bss = ctx.xn[i % 2], ctx.bxn[i % 2], ctx.ss[i % 2], ctx.bss[i % 2]
        K.op(ACT, lambda e: e.activation(out=ctx.junk[0:tp, :], in_=xt[0:tp, :], func=AF.Square,
                                         accum_out=ss[0:tp, 0:1]), reads=[bxt], writes=[bss, ctx.bjunk])
        K.op(ACT, lambda e: e.activation(out=ss[0:tp, 1:2], in_=ss[0:tp, 0:1], func=AF.Ln, scale=1.0 / D,
                                         bias=eps1[0:tp, :]), reads=[bss, cst], writes=[bss])
        K.op(ACT, lambda e: e.activation(out=ss[0:tp, 1:2], in_=ss[0:tp, 1:2], func=AF.Exp, scale=-0.5),
             reads=[bss], writes=[bss])
        K.op(DVE, lambda e: e.tensor_scalar(out=xn[0:tp, :], in0=xt[0:tp, :], scalar1=ss[0:tp, 1:2], scalar2=None,
                                            op0=ALU.mult), reads=[bxt, bss], writes=[bxn])
        return (xn, bxn)

    def emit_norm_tr(ctx, st_, tp, groups, Aa, Bb, hT_tile, bhT, tcol0, pbanks):
        xn, bxn = st_
        for half in range(2):
            pb = pbanks[half]
            pst = psb[pb][:].bitcast(BF16)
            for cc in range(8):
                c = half * 8 + cc
                K.op(PE, lambda e, c=c, cc=cc: e.transpose(out=pst[:, cc * 128:cc * 128 + tp],
                                                           in_=xn[0:tp, c * 128:(c + 1) * 128],
                                                           identity=ident[0:tp, 0:tp]),
                     reads=[bxn, cst], writes=[pbuf[pb]], signal=(cc == 7))
            for cc in range(8):
                c = half * 8 + cc
                for (s, g0, gn) in groups:
                    src = pst[:, cc * 128 + g0:cc * 128 + g0 + gn]
                    dst = hT_tile[:, c, tcol0 + g0:tcol0 + g0 + gn]
                    if half == 0:
                        K.op(ACT, lambda e, src=src, dst=dst, c=c, s=s: e.activation(
                            out=dst, in_=src, func=AF.Identity, scale=Aa[:, c, s:s + 1], bias=Bb[:, c, s:s + 1]),
                             reads=[pbuf[pb], bmod], writes=[bhT])
                    else:
                        K.op(DVE, lambda e, src=src, dst=dst, c=c, s=s: e.tensor_scalar(
                            out=dst, in0=src, scalar1=Aa[:, c, s:s + 1], scalar2=Bb[:, c, s:s + 1],
                            op0=ALU.mult, op1=ALU.add), reads=[pbuf[pb], bmod], writes=[bhT])

    with ExitStack() as st:
        def sb(name, shape, dt=F32):
            return st.enter_context(nc.sbuf_tensor(name, list(shape), dt))
        nctx = NormCtx(st, "a")
        xts = [sb(f"xt{i}", [128, D]) for i in range(3)]
        bxts = [Buf() for _ in range(3)]
        hTt = [sb(f"hTt{i}", [128, 16, 512], BF16) for i in range(2)]
        bhTt = [Buf(), Buf()]
        def p1_stats(tb):
            tp = 128 if tb < 32 else 64
            xt, bxt = xts[tb % 3], bxts[tb % 3]
            K.dma(SP, xt[0:tp, :], xall[tb * 128:tb * 128 + tp, :], writes=[bxt])
            return emit_norm_stats(nctx, xt, bxt, tp)
        pend = p1_stats(0)
        for nt in range(9):
            t0 = nt * 512
            nblk = 4 if nt < 8 else 1
            ht, bht = hTt[nt % 2], bhTt[nt % 2]
            ncols = 0
            for bi in range(nblk):
                tb = nt * 4 + bi
                tp = 128 if tb < 32 else 64
                cur = pend
                if tb + 1 < 33:
                    pend = p1_stats(tb + 1)
                emit_norm_tr(nctx, cur, tp, seq_groups(tb), A1, B1, ht, bht, bi * 128, (0 + 2 * (tb % 2), 1 + 2 * (tb % 2)))
                ncols += tp
            K.dma(POOL, hT_d[:, :, t0:t0 + ncols].rearrange("c p t -> p c t"), ht[:, :, 0:ncols], reads=[bht],
                  writes=[dbufs["hT"]])
        K.barrier()
    if stop_after <= 1:
        K.finish()
        return nc


    bankctr = [0]

    def nextbank():
        b = bankctr[0] % 8
        bankctr[0] += 1
        return b

    def proj_pass(is_ctx):
        tok0 = 0 if is_ctx else NCTX
        ntok = NCTX if is_ctx else NOWN
        ntiles = [(i * 512, 512) for i in range(4)] + ([] if is_ctx else [(2048, 64)])
        tblocks = [(i * 128, 128) for i in range(16)] + ([] if is_ctx else [(2048, 64)])
        with ExitStack() as st:
            def sb(name, shape, dt=F32):
                return st.enter_context(nc.sbuf_tensor(name + ("c" if is_ctx else "o"), list(shape), dt))
            hT_sb = sb("hT_sb", [128, 16, ntok], BF16)
            bhT = Buf()
            for c in range(16):
                K.dma(SP if c % 2 == 0 else POOL, hT_sb[:, c, :], hT_d[c, :, tok0:tok0 + ntok], reads=[dbufs["hT"]],
                      writes=[bhT])
            wst = [sb(f"wst{i}", [128, 16, 128]) for i in range(3)]
            wbf = [sb(f"wbf{i}", [128, 16, 128], BF16) for i in range(2)]
            bwst = [Buf() for _ in range(3)]
            bwbf = [Buf(), Buf()]
            cnt = [0]
            if is_ctx:
                fm_cols = [1024 + h * 128 for h in range(8)]
                for j in range(8):
                    fm_cols += [3072 + j * 128, 5120 + j * 128]
            else:
                fm_cols = [1024 + h * 128 for h in range(8)] + [h * 128 for h in range(8)]
                for j in range(8):
                    fm_cols += [3072 + j * 128, 5120 + j * 128, 4096 + j * 128]
                fm_cols += [6144 + j * 128 for j in range(16)] + [8192 + j * 128 for j in range(16)]
            loaded = [0]
            casted = [0]

            def fm_load_upto(k):
                while loaded[0] <= k and loaded[0] < len(fm_cols):
                    i = loaded[0]
                    c0 = fm_cols[i]
                    K.dma(SP, wst[i % 3][:], w_in[:, c0:c0 + 128].rearrange("(c p) n -> p c n", p=128),
                          writes=[bwst[i % 3]])
                    loaded[0] += 1

            def fm_cast_upto(k):
                while casted[0] <= k and casted[0] < len(fm_cols):
                    i = casted[0]
                    fm_load_upto(i)
                    K.op(DVE, lambda e, i=i: e.tensor_copy(out=wbf[i % 2][:], in_=wst[i % 3][:]), reads=[bwst[i % 3]],
                         writes=[bwbf[i % 2]])
                    casted[0] += 1

            def fm_block(col0, tiles, evac):
                i = cnt[0]
                cnt[0] += 1
                assert fm_cols[i] == col0, (i, fm_cols[i], col0)
                fm_cast_upto(i)
                fm_load_upto(i + 2)
                wb, bwb = wbf[i % 2], bwbf[i % 2]
                first_tile = True
                for (n0, n) in tiles:
                    pb = nextbank()
                    for c in range(16):
                        K.op(PE, lambda e, c=c: e.matmul(psb[pb][:, 0:n], lhsT=wb[:, c, :], rhs=hT_sb[:, c, n0:n0 + n],
                                                         start=(c == 0), stop=(c == 15)),
                             reads=[bwb, bhT], writes=[pbuf[pb]], signal=(c == 15))
                    evac(pb, n0, n)
                    if first_tile:
                        first_tile = False
                        fm_cast_upto(i + 1)

            with ExitStack() as st2:
                def sb2(name, shape, dt=F32):
                    return st2.enter_context(nc.sbuf_tensor(name + ("c" if is_ctx else "o"), list(shape), dt))
                obuf = [sb2(f"obuf{i}", [128, ntok], BF16) for i in range(4)]
                bobuf = [Buf() for _ in range(4)]
                octr = [0]

                def simple_block(col0, fn, dst_ap, dbuf):
                    oi = octr[0] % 4
                    octr[0] += 1
                    ob, bob = obuf[oi], bobuf[oi]

                    def evac(pb, n0, n):
                        fn(pb, n0, n, ob, bob)
                    fm_block(col0, ntiles, evac)
                    K.dma(POOL, dst_ap, ob[:, 0:ntok], reads=[bob], writes=[dbuf])

                def ev_copy_dve(pb, n0, n, ob, bob):
                    K.op(DVE, lambda e: e.tensor_copy(out=ob[:, n0:n0 + n], in_=psb[pb][:, 0:n]), reads=[pbuf[pb]],
                         writes=[bob])

                def ev_scale_act(pb, n0, n, ob, bob):
                    K.op(ACT, lambda e: e.activation(out=ob[:, n0:n0 + n], in_=psb[pb][:, 0:n], func=AF.Copy,
                                                     scale=float(128 ** -0.5)), reads=[pbuf[pb]], writes=[bob])

                def ev_sigmoid(pb, n0, n, ob, bob):
                    K.op(ACT, lambda e: e.activation(out=ob[:, n0:n0 + n], in_=psb[pb][:, 0:n], func=AF.Sigmoid),
                         reads=[pbuf[pb]], writes=[bob])

                import os
                CUT = int(os.environ.get("DBG_CUT", "99"))
                for h in range(8 if CUT > 0 else 1):
                    simple_block(1024 + h * 128, ev_copy_dve, KT_d[h, :, tok0:tok0 + ntok], dbufs["KT"])
                if CUT <= 1:
                    K.barrier()
                    return
                if is_ctx:
                    HC2 = sb2("HC2", [128, 8, 2])
                    bHC2 = Buf()
                    for j in range(8):
                        def ev_hc2(pb, n0, n, j=j):
                            K.op(DVE, lambda e: e.tensor_copy(out=HC2[:, j, :], in_=psb[pb][:, 0:2]), reads=[pbuf[pb]],
                                 writes=[bHC2])
                        fm_block(3072 + j * 128, [(2046, 2)], ev_hc2)

                        def ev_gc2(pb, n0, n, j=j):
                            K.op(DVE, lambda e: e.scalar_tensor_tensor(out=uprev[:, j, :], in0=psb[pb][:, 0:2],
                                                                       scalar=flg[:, 1:2], in1=HC2[:, j, :],
                                                                       op0=ALU.mult, op1=ALU.mult),
                                 reads=[pbuf[pb], bHC2, cst], writes=[buprev])
                        fm_block(5120 + j * 128, [(2046, 2)], ev_gc2)
                else:
                    for h in range(8):
                        simple_block(h * 128, ev_scale_act, QT_d[h, :, :], dbufs["QT"])
                    if CUT <= 2:
                        K.barrier()
                        return
                    HCt = [sb2(f"HCt{i}", [128, NOWN], BF16) for i in range(2)]
                    bHCt = [Buf(), Buf()]
                    Uf = [sb2(f"Uf{i}", [128, 2122]) for i in range(2)]
                    bUf = [Buf(), Buf()]
                    CV = [sb2(f"CV{i}", [128, NOWN]) for i in range(2)]
                    bCV = [Buf(), Buf()]
                    segs = [(0, 0, 2048)] + [(2050 + 18 * s_, 2048 + 16 * s_, 16) for s_ in range(4)]
                    for j in range(8):
                        hct, bhct, uf, buf_, cv, bcv = HCt[j % 2], bHCt[j % 2], Uf[j % 2], bUf[j % 2], CV[j % 2], bCV[j % 2]

                        def ev_hc(pb, n0, n):
                            K.op(ACT, lambda e: e.activation(out=hct[:, n0:n0 + n], in_=psb[pb][:, 0:n], func=AF.Copy),
                                 reads=[pbuf[pb]], writes=[bhct])
                        fm_block(3072 + j * 128, ntiles, ev_hc)
                        K.op(DVE, lambda e: e.tensor_copy(out=uf[:, 0:2], in_=uprev[:, j, :]), reads=[buprev],
                             writes=[buf_])
                        for s_ in range(4):
                            K.dma(POOL, uf[:, 2050 + 18 * s_:2052 + 18 * s_],
                                  state_conv[s_, :, j * 128:(j + 1) * 128].rearrange("r p -> p r"), writes=[buf_])

                        def ev_gc(pb, n0, n):
                            if n0 < 2048:
                                K.op(DVE, lambda e: e.tensor_tensor(out=uf[:, 2 + n0:2 + n0 + n], in0=psb[pb][:, 0:n],
                                                                    in1=hct[:, n0:n0 + n], op=ALU.mult),
                                     reads=[pbuf[pb], bhct], writes=[buf_])
                            else:
                                for s_ in range(4):
                                    K.op(DVE, lambda e, s_=s_: e.tensor_tensor(
                                        out=uf[:, 2052 + 18 * s_:2068 + 18 * s_], in0=psb[pb][:, 16 * s_:16 * s_ + 16],
                                        in1=hct[:, 2048 + 16 * s_:2064 + 16 * s_], op=ALU.mult),
                                         reads=[pbuf[pb], bhct], writes=[buf_])
                        fm_block(5120 + j * 128, ntiles, ev_gc)
                        for si, (uoff, coff, L) in enumerate(segs):
                            K.op(DVE, lambda e: e.tensor_scalar(out=cv[:, coff:coff + L], in0=uf[:, uoff:uoff + L],
                                                                 scalar1=cw[:, 0, j:j + 1], scalar2=None, op0=ALU.mult),
                                 reads=[buf_, cst], writes=[bcv])
                            for r in (1, 2):
                                K.op(DVE, lambda e, r=r: e.scalar_tensor_tensor(
                                    out=cv[:, coff:coff + L], in0=uf[:, uoff + r:uoff + r + L], scalar=cw[:, r, j:j + 1],
                                    in1=cv[:, coff:coff + L], op0=ALU.mult, op1=ALU.add), reads=[buf_, cst, bcv],
                                     writes=[bcv])
                            K.dma(POOL, nconv_out[si, :, j * 128:(j + 1) * 128].rearrange("r p -> p r"),
                                  uf[:, uoff + L:uoff + L + 2], reads=[buf_], is_output=True)

                        def ev_gb(pb, n0, n, ob, bob):
                            K.op(DVE, lambda e: e.tensor_tensor(out=ob[:, n0:n0 + n], in0=psb[pb][:, 0:n],
                                                                in1=cv[:, n0:n0 + n], op=ALU.mult),
                                 reads=[pbuf[pb], bcv], writes=[bob])
                        simple_block(4096 + j * 128, ev_gb, GC_d[j, :, :], dbufs["GC"])
                    if CUT <= 3:
                        K.barrier()
                        return
                    for j in range(16):
                        simple_block(6144 + j * 128, ev_sigmoid, SG_d[0, j, :, :], dbufs["SG"])
                    for j in range(16):
                        simple_block(8192 + j * 128, ev_sigmoid, SG_d[1, j, :, :], dbufs["SG"])
                K.barrier()
            if CUT <= 4 or (CUT == 5 and not is_ctx) or (CUT == 6 and is_ctx):
                return
            with ExitStack() as st3:
                def sb3(name, shape, dt=F32):
                    return st3.enter_context(nc.sbuf_tensor(name + ("c" if is_ctx else "o"), list(shape), dt))
                wst2 = [sb3(f"wst2{i}", [128, 16, 512]) for i in range(1)] * 2
                wbf2 = [sb3(f"wbf2{i}", [128, 16, 512], BF16) for i in range(2)]
                bwst2 = [Buf()] * 2
                bwbf2 = [Buf(), Buf()]
                stf = [sb3(f"stf{i}", [128, 512]) for i in range(4)]
                bstf = [Buf() for _ in range(4)]
                stb = [sb3(f"stb{i}", [128, 512], BF16) for i in range(4)]
                bstb = [Buf() for _ in range(4)]
                gi = 0
                ei = 0
                groups = ([] if is_ctx else [("k", g) for g in range(2)]) + [("v", g) for g in range(2)]
                for (kind, g) in groups:
                    col0 = (1024 if kind == "k" else 2048) + g * 512
                    ws, wb, bws, bwb = wst2[gi % 2], wbf2[gi % 2], bwst2[gi % 2], bwbf2[gi % 2]
                    gi += 1
                    K.dma(SP, ws[:], w_in[:, col0:col0 + 512].rearrange("(c p) n -> p c n", p=128), writes=[bws])
                    K.op(DVE, lambda e: e.tensor_copy(out=wb[:], in_=ws[:]), reads=[bws], writes=[bwb])
                    for (t0, tp) in tblocks:
                        pb = nextbank()
                        for c in range(16):
                            K.op(PE, lambda e, c=c: e.matmul(psb[pb][0:tp, 0:512], lhsT=hT_sb[:, c, t0:t0 + tp],
                                                             rhs=wb[:, c, :], start=(c == 0), stop=(c == 15)),
                                 reads=[bwb, bhT], writes=[pbuf[pb]], signal=(c == 15))
                        sf, bsf, sbb, bsb = stf[ei % 4], bstf[ei % 4], stb[ei % 4], bstb[ei % 4]
                        ei += 1
                        if not is_ctx:
                            K.op(DVE, lambda e: e.tensor_copy(out=sf[0:tp, :], in_=psb[pb][0:tp, 0:512]),
                                 reads=[pbuf[pb]], writes=[bsf])
                            dst = nk_out if kind == "k" else nv_out
                            K.dma(POOL, dst[t0:t0 + tp, g * 512:(g + 1) * 512], sf[0:tp, :], reads=[bsf], is_output=True)
                        if kind == "v":
                            K.op(ACT, lambda e: e.activation(out=sbb[0:tp, :], in_=psb[pb][0:tp, 0:512], func=AF.Copy),
                                 reads=[pbuf[pb]], writes=[bsb])
                            K.dma(POOL, V_d[tok0 + t0:tok0 + t0 + tp, g * 512:(g + 1) * 512], sbb[0:tp, :], reads=[bsb],
                                  writes=[dbufs["V"]])
                K.barrier()

    proj_pass(True)
    proj_pass(False)
    if stop_after <= 2:
        K.finish()
        return nc


    def run_sb_pipeline(st, tag, tiles, classes):
        def sb(name, shape, dt=F32):
            return st.enter_context(nc.sbuf_tensor(name + tag, list(shape), dt))
        NE = 4
        pools = {}
        for cls, (nq_, banks_) in classes.items():
            pools[cls] = dict(
                nq=nq_, banks=banks_, ctr=0,
                ebuf=[sb(f"ebuf{cls}{i}", [128, nq_]) for i in range(NE)], bebuf=[Buf() for _ in range(NE)],
                Lb=[sb(f"Lb{cls}{i}", [128, nq_], BF16) for i in range(NE)], bLb=[Buf() for _ in range(NE)],
                ab=[sb(f"ab{cls}{i}", [128, nq_], BF16) for i in range(NE)], bab=[Buf() for _ in range(NE)])
        n = len(tiles)
        for t in tiles:
            P_ = pools[t["cls"]]
            t["k"] = P_["ctr"]
            P_["ctr"] += 1

        def stS(i):
            t = tiles[i]
            P_ = pools[t["cls"]]
            t["bk"] = P_["banks"][t["k"] % len(P_["banks"])]
            t["s_mm"](t["bk"])

        def stA(i):
            t = tiles[i]
            P_ = pools[t["cls"]]
            nq, k = P_["nq"], t["k"]
            bk, nk = t["bk"], t["nkeys"]
            eb, beb, lb, blb = P_["ebuf"][k % NE], P_["bebuf"][k % NE], P_["Lb"][k % NE], P_["bLb"][k % NE]
            bias_ap = t["bias"]
            K.op(ACT, lambda e: e.activation(out=eb[0:nk, :], in_=psb[bk][0:nk, 0:nq], func=AF.Exp,
                                             bias=bias_ap[0:nk, :]), reads=[pbuf[bk], cst], writes=[beb])
            K.op(ACT, lambda e: e.activation(out=lb[0:nk, :], in_=eb[0:nk, :], func=AF.Ln, bias=1.0),
                 reads=[beb], writes=[blb])
            if t["mask"] is not None:
                K.op(POOL, lambda e: e.tensor_tensor(out=lb[0:nk, :], in0=lb[0:nk, :], in1=t["mask"], op=ALU.mult),
                     reads=[blb, cst], writes=[blb])
            Lacc, bLacc, Laccb, bLaccb = t["lacc"]
            first = t["first"]
            rd = [blb, cst]
            if not first:
                ci = t["ci"]
                lab, blab = Laccb[ci % 2], bLaccb[ci % 2]
                K.op(DVE, lambda e: e.tensor_copy(out=lab[:, :], in_=Lacc[:, :]), reads=[bLacc], writes=[blab])
                rd.append(blab)
            K.op(PE, lambda e: e.matmul(psb[bk][0:nk, 0:nq], lhsT=nti[0:nk, 0:nk], rhs=lb[0:nk, :],
                                        start=False, stop=first, skip_group_check=True),
                 reads=rd, writes=[pbuf[bk]], signal=first)
            if not first:
                K.op(PE, lambda e: e.matmul(psb[bk][0:nk, 0:nq], lhsT=neg1[:, 0:nk], rhs=lab[:, :],
                                            start=False, stop=True, skip_group_check=True),
                     reads=rd, writes=[pbuf[bk]])
            if first:
                if nk < 128:
                    K.op(DVE, lambda e: e.memset(Lacc[:, :], 0.0), writes=[bLacc])
                K.op(DVE, lambda e: e.tensor_copy(out=Lacc[0:nk, :], in_=lb[0:nk, :]), reads=[blb], writes=[bLacc])
            else:
                K.op(DVE, lambda e: e.tensor_tensor(out=Lacc[0:nk, :], in0=Lacc[0:nk, :], in1=lb[0:nk, :], op=ALU.add),
                     reads=[blb, bLacc], writes=[bLacc])

        def stB(i):
            t = tiles[i]
            P_ = pools[t["cls"]]
            nq, k = P_["nq"], t["k"]
            bk, nk = t["bk"], t["nkeys"]
            aa, baa = P_["ab"][k % NE], P_["bab"][k % NE]
            bias_ap = t["bias"]
            K.op(ACT, lambda e: e.activation(out=aa[0:nk, :], in_=psb[bk][0:nk, 0:nq], func=AF.Exp,
                                             bias=bias_ap[0:nk, :]), reads=[pbuf[bk], cst], writes=[baa])
            if t["mask"] is not None:
                K.op(POOL, lambda e: e.tensor_tensor(out=aa[0:nk, :], in0=aa[0:nk, :], in1=t["mask"], op=ALU.mult),
                     reads=[baa, cst], writes=[baa])
            t["av_mm"](aa, baa)
            if t.get("on_last") is not None:
                t["on_last"]()

        LA = 12
        for s_ in range(-LA, n):
            if 0 <= s_ + LA < n and tiles[s_ + LA].get("prep") is not None:
                tiles[s_ + LA]["prep"]()
            if 0 <= s_ + 2 < n:
                stS(s_ + 2)
            if 0 <= s_ + 1 < n:
                stA(s_ + 1)
            if 0 <= s_ < n:
                stB(s_)

    with ExitStack() as st:
        def sb(name, shape, dt=F32):
            return st.enter_context(nc.sbuf_tensor(name, list(shape), dt))
        KTh = [sb(f"KTh{i}", [128, 4096], BF16) for i in range(2)]
        Vh = [sb(f"Vh{i}", [128, 32, 128], BF16) for i in range(2)]
        QTh = [sb(f"QTh{i}", [128, 2048], BF16) for i in range(2)]
        bKV = [Buf(), Buf()]
        LaccP = [sb(f"LaccP{i}", [128, 512]) for i in range(2)]
        bLaccP = [Buf(), Buf()]
        LaccbP = [[sb(f"LaccbP{i}{j}", [128, 512], BF16) for j in range(2)] for i in range(2)]
        bLaccbP = [[Buf(), Buf()], [Buf(), Buf()]]
        ot = [sb(f"ot{i}", [128, 512], BF16) for i in range(2)]
        bot = [Buf(), Buf()]
        tiles = []
        qctr = 0
        for h in range(8):
            kt, vh, qt, bkv = KTh[h % 2], Vh[h % 2], QTh[h % 2], bKV[h % 2]

            def prep_head(h=h, kt=kt, vh=vh, qt=qt, bkv=bkv):
                K.dma(SP, kt[:], KT_d[h, :, 0:4096], reads=[dbufs["KT"]], writes=[bkv])
                K.dma(SP, vh[:], V_d[0:4096, h * 128:(h + 1) * 128].rearrange("(b p) d -> p b d", p=128),
                      reads=[dbufs["V"]], writes=[bkv])
                K.dma(SP, qt[:], QT_d[h, :, 0:2048], reads=[dbufs["QT"]], writes=[bkv])
            for qti in range(4):
                ob = 3 + (qctr % 2)
                li = qctr % 2
                qctr += 1
                q0 = qti * 512
                nkb = 16 + 4 * qti + 4
                for r, kb in enumerate(range(nkb - 1, -1, -1)):
                    first = (r == 0)
                    last = (kb == 0)
                    m = kb - (16 + 4 * qti)

                    def s_mm(bk, kb=kb, kt=kt, qt=qt, bkv=bkv, q0=q0):
                        K.op(PE, lambda e: e.matmul(psb[bk][:, :], lhsT=kt[:, kb * 128:(kb + 1) * 128],
                                                    rhs=qt[:, q0:q0 + 512], start=True, stop=False,
                                                    skip_group_check=True), reads=[bkv], writes=[pbuf[bk]])

                    def av_mm(aa, baa, kb=kb, first=first, last=last, vh=vh, bkv=bkv, ob=ob):
                        K.op(PE, lambda e: e.matmul(psb[ob][:, :], lhsT=vh[:, kb, :], rhs=aa[:, :], start=first,
                                                    stop=last, skip_group_check=True), reads=[bkv, baa],
                             writes=[pbuf[ob]])

                    def on_last(h=h, q0=q0, ob=ob, qti=qti):
                        o_t, bo_t = ot[qti % 2], bot[qti % 2]
                        K.op(DVE, lambda e: e.tensor_copy(out=o_t[:], in_=psb[ob][:, :]), reads=[pbuf[ob]],
                             writes=[bo_t])
                        K.dma(POOL, OT_d[h, :, q0:q0 + 512], o_t[:], reads=[bo_t], writes=[dbufs["OT"]])
                    tiles.append(dict(
                        cls="p", nkeys=128, first=first, ci=r, mask=(maskd[:, m, :] if m >= 0 else None),
                        bias=(flg[:, 0:1] if kb < 16 else zero1), s_mm=s_mm, av_mm=av_mm,
                        lacc=(LaccP[li], bLaccP[li], LaccbP[li], bLaccbP[li]),
                        on_last=(on_last if last else None),
                        prep=(prep_head if (qti == 0 and first) else None)))
        ptiles = tiles
        LaccS = [sb(f"LaccS{i}", [128, 128]) for i in range(4)]
        bLaccS = [Buf() for _ in range(4)]
        LaccbS = [[sb(f"LaccbS{i}{j}", [128, 128], BF16) for j in range(2)] for i in range(4)]
        bLaccbS = [[Buf(), Buf()] for _ in range(4)]
        NKF = 4
        kf = [sb(f"kf{i}", [128, 1024]) for i in range(NKF)]
        vf = [sb(f"vf{i}", [128, 1024]) for i in range(NKF)]
        bkf = [Buf() for _ in range(NKF)]
        bvf = [Buf() for _ in range(NKF)]
        kbf = [sb(f"kbf{i}", [128, 1024], BF16) for i in range(3)]
        bkbf = [Buf() for _ in range(3)]
        NV = 8
        vbf = [sb(f"vbf{i}", [128, 1024], BF16) for i in range(NV)]
        bvbf = [Buf() for _ in range(NV)]
        ktb = [sb(f"ktb{i}", [128, 8, 128], BF16) for i in range(NV)]
        bktb = [Buf() for _ in range(NV)]
        qs = sb("qs", [128, 8, 64], BF16)
        bqs = Buf()
        ktn = sb("ktn", [128, 8, 64], BF16)
        vn = sb("vn", [16, 4, 1024], BF16)
        bnew = Buf()
        ots = sb("ots", [128, 4, 128], BF16)
        bots = Buf()
        for h in range(8):
            K.dma(SP, qs[:, h, :], QT_d[h, :, 2048:2112], reads=[dbufs["QT"]], writes=[bqs])
            K.dma(SP, ktn[:, h, :], KT_d[h, :, 4096:4160], reads=[dbufs["KT"]], writes=[bnew])
        for sq in range(4):
            K.dma(SP, vn[:, sq, :], V_d[4096 + 16 * sq:4112 + 16 * sq, :], reads=[dbufs["V"]], writes=[bnew])
        OB = 7
        tiles = []
        li = 0
        for r in range(33):
            for sq in range(4):
                first = (r == 0)
                last = (r == 32)
                if first:
                    nkeys = 16
                    mask_ap = masks[:].rearrange("p h i -> p (h i)")
                    prep = None
                    kti = bkti = vbi = bvbi = None
                else:
                    kb = 32 - r
                    nkeys = 128
                    mask_ap = None
                    kfi, bkfi, vfi, bvfi = kf[li % NKF], bkf[li % NKF], vf[li % NKF], bvf[li % NKF]
                    kbi, bkbi = kbf[li % 3], bkbf[li % 3]
                    vbi, bvbi = vbf[li % NV], bvbf[li % NV]
                    kti, bkti = ktb[li % NV], bktb[li % NV]
                    tbk = 5
                    li += 1

                    def prep(sq=sq, kb=kb, kfi=kfi, bkfi=bkfi, vfi=vfi, bvfi=bvfi, kbi=kbi, bkbi=bkbi, vbi=vbi,
                             bvbi=bvbi, kti=kti, bkti=bkti, tbk=tbk):
                        K.dma(SP, kfi[:], cache_k[sq, kb * 128:(kb + 1) * 128, :], writes=[bkfi])
                        K.dma(POOL, vfi[:], cache_v[sq, kb * 128:(kb + 1) * 128, :], writes=[bvfi])
                        K.op(DVE, lambda e: e.tensor_copy(out=kbi[:], in_=kfi[:]), reads=[bkfi], writes=[bkbi])
                        K.op(ACT, lambda e: e.activation(out=vbi[:], in_=vfi[:], func=AF.Copy), reads=[bvfi],
                             writes=[bvbi])
                        pst = psb[tbk][:].bitcast(BF16)
                        for h in range(8):
                            K.op(PE, lambda e, h=h: e.transpose(out=pst[:, h * 128:(h + 1) * 128],
                                                                in_=kbi[:, h * 128:(h + 1) * 128], identity=ident[:]),
                                 reads=[bkbi, cst], writes=[pbuf[tbk]], signal=(h == 7))
                        K.op(DVE, lambda e: e.tensor_copy(out=kti[:].rearrange("p h k -> p (h k)"), in_=pst[:, :]),
                             reads=[pbuf[tbk]], writes=[bkti])

                def s_mm(bk, first=first, sq=sq, nkeys=nkeys, kti=kti, bkti=bkti):
                    for h in range(8):
                        if first:
                            lhsT = ktn[:, h, 16 * sq:16 * sq + 16]
                            rds = [bnew, bqs]
                        else:
                            lhsT = kti[:, h, :]
                            rds = [bkti, bqs]
                        K.op(PE, lambda e, h=h, lhsT=lhsT: e.matmul(psb[bk][0:nkeys, h * 16:(h + 1) * 16], lhsT=lhsT,
                                                                    rhs=qs[:, h, 16 * sq:16 * sq + 16], start=(h == 0),
                                                                    stop=False, skip_group_check=True),
                             reads=rds, writes=[pbuf[bk]], signal=(h == 7))

                def av_mm(aa, baa, first=first, last=last, sq=sq, nkeys=nkeys, vbi=vbi, bvbi=bvbi, r=r):
                    for h in range(8):
                        if first:
                            lhsT = vn[0:16, sq, h * 128:(h + 1) * 128]
                            rds = [bnew, baa]
                        else:
                            lhsT = vbi[:, h * 128:(h + 1) * 128]
                            rds = [bvbi, baa]
                        c0 = sq * 128 + h * 16
                        K.op(PE, lambda e, h=h, lhsT=lhsT, c0=c0: e.matmul(
                            psb[OB][:, c0:c0 + 16], lhsT=lhsT, rhs=aa[0:nkeys, h * 16:(h + 1) * 16],
                            start=(first and sq == 0 and h == 0), stop=last, skip_group_check=True),
                             reads=rds, writes=[pbuf[OB]], signal=(h == 7))

                def on_last_all():
                    K.op(DVE, lambda e: e.tensor_copy(out=ots[:].rearrange("p s c -> p (s c)"), in_=psb[OB][:, :]),
                         reads=[pbuf[OB]], writes=[bots])
                    for h in range(8):
                        for sq_ in range(4):
                            K.dma(POOL, OT_d[h, :, 2048 + 16 * sq_:2064 + 16 * sq_], ots[:, sq_, h * 16:(h + 1) * 16],
                                  reads=[bots], writes=[dbufs["OT"]])
                tiles.append(dict(
                    cls="s", nkeys=nkeys, first=first, ci=r, mask=mask_ap, bias=zero1, s_mm=s_mm, av_mm=av_mm,
                    lacc=(LaccS[sq], bLaccS[sq], LaccbS[sq], bLaccbS[sq]),
                    on_last=(on_last_all if (last and sq == 3) else None), prep=prep))
        stiles = tiles
        merged = []
        si_ = 0
        for pi_, t_ in enumerate(ptiles):
            merged.append(t_)
            if pi_ % 6 == 5 and si_ < len(stiles):
                merged.append(stiles[si_])
                si_ += 1
        merged += stiles[si_:]
        run_sb_pipeline(st, "m", merged, {"p": (512, (0, 1, 2, 6)), "s": (128, (5,))})
        K.barrier()
    if stop_after <= 3:
        K.finish()
        return nc

    own_tiles = [(i * 512, 512) for i in range(4)] + [(2048, 64)]
    own_blocks = [(i * 128, 128) for i in range(16)] + [(2048, 64)]
    with ExitStack() as st:
        def sb(name, shape, dt=F32):
            return st.enter_context(nc.sbuf_tensor(name, list(shape), dt))
        OT_sb = sb("OT_sb", [128, 8, NOWN], BF16)
        GC_sb = sb("GC_sb", [128, 8, NOWN], BF16)
        bOG = Buf()
        for c in range(8):
            K.dma(SP, OT_sb[:, c, :], OT_d[c, :, :], reads=[dbufs["OT"]], writes=[bOG])
            K.dma(POOL, GC_sb[:, c, :], GC_d[c, :, :], reads=[dbufs["GC"]], writes=[bOG])
        wf = [sb(f"wf{i}", [128, 2, 8, 128]) for i in range(2)]
        wb_ = [sb(f"wb{i}", [128, 2, 8, 128], BF16) for i in range(2)]
        bwf = [Buf(), Buf()]
        bwb = [Buf(), Buf()]
        sg = [sb(f"sg{i}", [128, 2, NOWN], BF16) for i in range(2)]
        bsg = [Buf(), Buf()]
        mgt = [sb(f"mgt{i}", [128, NOWN], BF16) for i in range(2)]
        bmgt = [Buf(), Buf()]
        t1 = [sb(f"t1{i}", [128, 512]) for i in range(2)]
        t2 = [sb(f"t2{i}", [128, 512]) for i in range(2)]
        bt1 = [Buf(), Buf()]
        bt2 = [Buf(), Buf()]
        ti = 0
        for j in range(16):
            f_, b_, bf_, bb_ = wf[j % 2], wb_[j % 2], bwf[j % 2], bwb[j % 2]
            K.dma(SP, f_[:, 0, :, :], w_ap[:, j * 128:(j + 1) * 128].rearrange("(c p) n -> p c n", p=128), writes=[bf_])
            K.dma(SP, f_[:, 1, :, :], w_co[:, j * 128:(j + 1) * 128].rearrange("(c p) n -> p c n", p=128), writes=[bf_])
            K.op(ACT, lambda e: e.activation(out=b_[:], in_=f_[:], func=AF.Copy), reads=[bf_], writes=[bb_])
            sg_, bsg_ = sg[j % 2], bsg[j % 2]
            K.dma(SP, sg_[:, 0, :], SG_d[0, j, :, :], reads=[dbufs["SG"]], writes=[bsg_])
            K.dma(SP, sg_[:, 1, :], SG_d[1, j, :, :], reads=[dbufs["SG"]], writes=[bsg_])
            mg_, bmg_ = mgt[j % 2], bmgt[j % 2]
            for (n0, n) in own_tiles:
                pa, pc = nextbank(), nextbank()
                for c in range(8):
                    K.op(PE, lambda e, c=c: e.matmul(psb[pa][:, 0:n], lhsT=b_[:, 0, c, :], rhs=OT_sb[:, c, n0:n0 + n],
                                                     start=(c == 0), stop=(c == 7)), reads=[bb_, bOG],
                         writes=[pbuf[pa]], signal=(c == 7))
                for c in range(8):
                    K.op(PE, lambda e, c=c: e.matmul(psb[pc][:, 0:n], lhsT=b_[:, 1, c, :], rhs=GC_sb[:, c, n0:n0 + n],
                                                     start=(c == 0), stop=(c == 7)), reads=[bb_, bOG],
                         writes=[pbuf[pc]], signal=(c == 7))
                a1, a2, ba1, ba2 = t1[ti % 2], t2[ti % 2], bt1[ti % 2], bt2[ti % 2]
                ti += 1
                K.op(DVE, lambda e: e.tensor_tensor(out=a1[:, 0:n], in0=psb[pa][:, 0:n], in1=sg_[:, 0, n0:n0 + n],
                                                    op=ALU.mult), reads=[pbuf[pa], bsg_], writes=[ba1])
                K.op(DVE, lambda e: e.tensor_tensor(out=a2[:, 0:n], in0=psb[pc][:, 0:n], in1=sg_[:, 1, n0:n0 + n],
                                                    op=ALU.mult), reads=[pbuf[pc], bsg_], writes=[ba2])
                K.op(POOL, lambda e: e.tensor_tensor(out=mg_[:, n0:n0 + n], in0=a1[:, 0:n], in1=a2[:, 0:n], op=ALU.add),
                     reads=[ba1, ba2], writes=[bmg_])
            K.dma(POOL, MG_d[j, :, :], mg_[:], reads=[bmg_], writes=[dbufs["MG"]])
        K.barrier()
    with ExitStack() as st:
        def sb(name, shape, dt=F32):
            return st.enter_context(nc.sbuf_tensor(name, list(shape), dt))
        wob = sb("wob", [128, 16, D], BF16)
        bwob = Buf()
        stg = [sb(f"stg{i}", [128, D]) for i in range(2)]
        bstg = [Buf(), Buf()]
        for c in range(16):
            K.dma(SP, stg[c % 2][:], w_o[c * 128:(c + 1) * 128, :], writes=[bstg[c % 2]])
            K.op(DVE if c % 2 == 0 else ACT,
                 (lambda e, c=c: e.tensor_copy(out=wob[:, c, :], in_=stg[c % 2][:])) if c % 2 == 0 else
                 (lambda e, c=c: e.activation(out=wob[:, c, :], in_=stg[c % 2][:], func=AF.Copy)),
                 reads=[bstg[c % 2]], writes=[bwob])
        G1 = sb("G1", [128, D])
        bG1 = Buf()
        K.dma(SP, G1[:], gS_d[0, 0, :].unsqueeze(0).to_broadcast([128, D]), reads=[dbufs["gS"]], writes=[bG1])
        xts = [sb(f"x4t{i}", [128, D]) for i in range(2)]
        bxts = [Buf(), Buf()]
        tq = [sb(f"tq{i}", [128, 512]) for i in range(2)]
        btq = [Buf(), Buf()]
        mgl = [sb(f"mgl{i}", [128, 16, 128], BF16) for i in range(2)]
        bmgl = [Buf(), Buf()]
        h2t = [sb(f"h2t{i}", [128, 16, 128], BF16) for i in range(2)]
        bh2t = [Buf(), Buf()]
        nctx2 = NormCtx(st, "b")
        qi = 0
        def p4_front(tb):
            nonlocal_qi = qi_box
            t0, tp = own_blocks[tb]
            if tb == 16:
                for s_ in range(4):
                    K.dma(SP, G1[16 * s_:16 * s_ + 16, :], gS_d[0, 1 + s_, :].unsqueeze(0).to_broadcast([16, D]),
                          reads=[dbufs["gS"]], writes=[bG1])
            ml, bml = mgl[tb % 2], bmgl[tb % 2]
            K.dma(SP, ml[:, :, 0:tp], MG_d[:, :, t0:t0 + tp].rearrange("c p t -> p c t"), reads=[dbufs["MG"]],
                  writes=[bml])
            xt, bxt = xts[tb % 2], bxts[tb % 2]
            K.dma(SP, xt[0:tp, :], xall[NCTX + t0:NCTX + t0 + tp, :], writes=[bxt])
            for nq in range(4):
                pb = nq
                for c in range(16):
                    K.op(PE, lambda e, c=c: e.matmul(psb[pb][0:tp, :], lhsT=ml[:, c, 0:tp],
                                                     rhs=wob[:, c, nq * 512:(nq + 1) * 512], start=(c == 0),
                                                     stop=(c == 15)), reads=[bml, bwob], writes=[pbuf[pb]],
                         signal=(c == 15))
                tt, btt = tq[nonlocal_qi[0] % 2], btq[nonlocal_qi[0] % 2]
                nonlocal_qi[0] += 1
                K.op(DVE, lambda e: e.tensor_tensor(out=tt[0:tp, :], in0=psb[pb][0:tp, :],
                                                    in1=G1[0:tp, nq * 512:(nq + 1) * 512], op=ALU.mult),
                     reads=[pbuf[pb], bG1], writes=[btt])
                K.op(POOL, lambda e: e.tensor_tensor(out=xt[0:tp, nq * 512:(nq + 1) * 512],
                                                     in0=xt[0:tp, nq * 512:(nq + 1) * 512], in1=tt[0:tp, :], op=ALU.add),
                     reads=[btt, bxt], writes=[bxt])
            K.dma(POOL, X1_d[t0:t0 + tp, :], xt[0:tp, :], reads=[bxt], writes=[dbufs["X1"]])
            return emit_norm_stats(nctx2, xt, bxt, tp)

        def p4_back(tb, st_):
            t0, tp = own_blocks[tb]
            ht, bht = h2t[tb % 2], bh2t[tb % 2]
            emit_norm_tr(nctx2, st_, tp, seq_groups(16 + tb), A2, B2, ht, bht, 0, (4 + 2 * (tb % 2), 5 + 2 * (tb % 2)))
            K.dma(POOL, H2_d[:, :, t0:t0 + tp].rearrange("c p t -> p c t"), ht[:, :, 0:tp], reads=[bht],
                  writes=[dbufs["H2"]])

        qi_box = [0]
        pend = p4_front(0)
        for tb in range(len(own_blocks)):
            cur = pend
            if tb + 1 < len(own_blocks):
                pend = p4_front(tb + 1)
            p4_back(tb, cur)
        K.barrier()
    if stop_after <= 4:
        K.finish()
        return nc


    with ExitStack() as st:
        def sb(name, shape, dt=F32):
            return st.enter_context(nc.sbuf_tensor(name, list(shape), dt))
        h2T = sb("h2T", [128, 16, NOWN], BF16)
        bh2T = Buf()
        for c in range(16):
            K.dma(SP if c % 2 == 0 else POOL, h2T[:, c, :], H2_d[c, :, :], reads=[dbufs["H2"]], writes=[bh2T])
        identf = sb("identf", [128, 128])
        bidf = Buf()
        K.op(POOL, lambda e: e.memset(identf[:], 0.0), writes=[bidf])
        K.op(POOL, lambda e: e.affine_select(out=identf[:], in_=identf[:], pattern=[[-1, 128]],
                                             compare_op=ALU.not_equal, fill=1.0, base=0, channel_multiplier=1),
             reads=[bidf], writes=[bidf])
        sk_sb = sb("sk_sb", [128, 16, 128])
        bsk = Buf()
        K.dma(SP, sk_sb[:], sub_keys.rearrange("g k d -> k g d"), writes=[bsk])
        SKT = sb("SKT", [128, 16, 128])
        bSKT = Buf()
        for g4 in range(4):
            pb = nextbank()
            for gg in range(4):
                g = g4 * 4 + gg
                K.op(PE, lambda e, g=g, gg=gg: e.matmul(psb[pb][:, gg * 128:(gg + 1) * 128], lhsT=sk_sb[:, g, :],
                                                        rhs=identf[:], start=True, stop=True),
                     reads=[bsk, bidf], writes=[pbuf[pb]], signal=(gg == 3))
            K.op(DVE, lambda e: e.tensor_copy(out=SKT[:, g4 * 4:(g4 + 1) * 4, :].rearrange("p g k -> p (g k)"),
                                              in_=psb[pb][:, :]), reads=[pbuf[pb]], writes=[bSKT])
        wqf = [sb(f"wqf{i}", [128, 16, 128]) for i in range(2)]
        wqb = [sb(f"wqb{i}", [128, 16, 128], BF16) for i in range(2)]
        bwqf = [Buf(), Buf()]
        bwqb = [Buf(), Buf()]
        qTg = [sb(f"qTg{i}", [128, NOWN]) for i in range(2)]
        bqTg = [Buf(), Buf()]
        sst = [sb(f"sst{i}", [128, 17, 128]) for i in range(2)]
        bsst = [Buf(), Buf()]
        for g in range(16):
            f_, b_, bf_, bb_ = wqf[g % 2], wqb[g % 2], bwqf[g % 2], bwqb[g % 2]
            K.dma(SP, f_[:], w_q[:, g * 128:(g + 1) * 128].rearrange("(c p) n -> p c n", p=128), writes=[bf_])
            K.op(DVE, lambda e: e.tensor_copy(out=b_[:], in_=f_[:]), reads=[bf_], writes=[bb_])
            qg, bqg = qTg[g % 2], bqTg[g % 2]
            for (n0, n) in own_tiles:
                pb = nextbank()
                for c in range(16):
                    K.op(PE, lambda e, c=c: e.matmul(psb[pb][:, 0:n], lhsT=b_[:, c, :], rhs=h2T[:, c, n0:n0 + n],
                                                     start=(c == 0), stop=(c == 15)), reads=[bb_, bh2T],
                         writes=[pbuf[pb]], signal=(c == 15))
                K.op(ACT, lambda e: e.activation(out=qg[:, n0:n0 + n], in_=psb[pb][:, 0:n], func=AF.Copy),
                     reads=[pbuf[pb]], writes=[bqg])
            ss_, bss_ = sst[g % 2], bsst[g % 2]
            for b4 in range(5):
                pb = nextbank()
                blks = own_blocks[b4 * 4:(b4 + 1) * 4]
                for bi, (t0, tp) in enumerate(blks):
                    K.op(PE, lambda e, bi=bi, t0=t0, tp=tp: e.matmul(psb[pb][0:tp, bi * 128:(bi + 1) * 128],
                                                                     lhsT=qg[:, t0:t0 + tp], rhs=SKT[:, g, :],
                                                                     start=True, stop=True),
                         reads=[bqg, bSKT], writes=[pbuf[pb]], signal=(bi == len(blks) - 1))
                tpm = blks[0][1]
                nb = len(blks)
                K.op(DVE, lambda e: e.tensor_copy(out=ss_[0:tpm, b4 * 4:b4 * 4 + nb, :],
                                                  in_=psb[pb][0:tpm, 0:nb * 128].rearrange("p (b k) -> p b k", b=nb)),
                     reads=[pbuf[pb]], writes=[bss_])
            K.dma(POOL, S_d[0:16, :, g * 128:(g + 1) * 128].rearrange("b p k -> p b k"), ss_[:, 0:16, :], reads=[bss_],
                  writes=[dbufs["S"]])
            K.dma(POOL, S_d[16, 0:64, g * 128:(g + 1) * 128], ss_[0:64, 16, :], reads=[bss_], writes=[dbufs["S"]])
        K.barrier()
    if stop_after <= 5:
        K.finish()
        return nc

    with ExitStack() as st:
        def sb(name, shape, dt=F32):
            return st.enter_context(nc.sbuf_tensor(name, list(shape), dt))
        s_sbs = [sb(f"s_sb{i}", [128, 16, 128]) for i in range(2)]
        bss_ = [Buf(), Buf()]
        v16 = sb("v16", [128, 16, 16])
        bv = Buf()
        tmpA2 = [sb(f"tmpA{i}", [128, 128]) for i in range(2)]
        btA2 = [Buf(), Buf()]
        joinj = sb("joinj", [128, 1])
        bvg = [Buf() for _ in range(16)]
        bcth = [Buf() for _ in range(8)]
        cand = sb("cand", [128, 8, 256])
        bcand = Buf()
        tmpB2 = [sb(f"tmpB{i}", [128, 256]) for i in range(2)]
        btB2 = [Buf(), Buf()]
        ctop = sb("ctop", [128, 8, 16])
        cidx = sb("cidx", [128, 8, 16], U32)
        bct = Buf()
        gate = sb("gate", [128, 8, 16])
        zs = sb("zs", [128, 8])
        bgate = Buf()
        iu = sb("iu", [128, 8, 16], U32)
        ju = sb("ju", [128, 8, 16], U32)
        i_f = sb("i_f", [128, 8, 16])
        j_f = sb("j_f", [128, 8, 16])
        bij = Buf()
        oh = sb("oh", [128, 8, 16, 16])
        boh = Buf()
        vsel = sb("vsel", [128, 2, 8, 16])
        bvsel = Buf()
        E1s = [sb(f"E1_{i}", [128, 8, 16, 32], BF16) for i in range(2)]
        E2s = [sb(f"E2_{i}", [128, 8, 16, 32], BF16) for i in range(2)]
        bE1s = [Buf(), Buf()]
        bE2s = [Buf(), Buf()]
        XT1 = sb("XT1", [128, 128, 128], BF16)
        XT2 = sb("XT2", [128, 128, 128], BF16)
        bXT1 = Buf()
        bXT2 = Buf()
        Gsb = sb("Gsb", [128, 128, 128], BF16)
        bGsb = Buf()
        K.op(POOL, lambda e: e.memset(Gsb[:], 0.0), writes=[bGsb])
        ectr = 0
        for tb, (t0, tp) in enumerate(own_blocks):
            s_sb, bs = s_sbs[tb % 2], bss_[tb % 2]
            if tb == 0:
                K.dma(SP, s_sb[0:tp, :, :].rearrange("p g k -> p (g k)"), S_d[0, 0:tp, :], reads=[dbufs["S"]], writes=[bs])
            if tb + 1 < len(own_blocks):
                tpn = own_blocks[tb + 1][1]
                K.dma(SP, s_sbs[(tb + 1) % 2][0:tpn, :, :].rearrange("p g k -> p (g k)"), S_d[tb + 1, 0:tpn, :],
                      reads=[dbufs["S"]], writes=[bss_[(tb + 1) % 2]])
            for g0 in range(8):
                pair = (g0, g0 + 8)
                for k_, g in enumerate(pair):
                    K.op(DVE, lambda e, g=g: e.max(out=v16[0:tp, g, 0:8], in_=s_sb[0:tp, g, :]), reads=[bs],
                         writes=[bvg[g]])
                for k_, g in enumerate(pair):
                    K.op(DVE, lambda e, g=g, k_=k_: e.match_replace(out=tmpA2[k_][0:tp, :], in_to_replace=v16[0:tp, g, 0:8],
                                                                    in_values=s_sb[0:tp, g, :], imm_value=NEG),
                         reads=[bs, bvg[g]], writes=[btA2[k_]])
                for k_, g in enumerate(pair):
                    K.op(DVE, lambda e, g=g, k_=k_: e.max(out=v16[0:tp, g, 8:16], in_=tmpA2[k_][0:tp, :]),
                         reads=[btA2[k_]], writes=[bvg[g]])
            vv = v16[0:tp, :, :].rearrange("p (h two) n -> p h two n", two=2)
            K.op(DVE, lambda e: e.tensor_tensor(out=cand[0:tp, :, :].rearrange("p h (i j) -> p h i j", i=16),
                                                in0=vv[:, :, 0, :].unsqueeze(3).to_broadcast([tp, 8, 16, 16]),
                                                in1=vv[:, :, 1, :].unsqueeze(2).to_broadcast([tp, 8, 16, 16]),
                                                op=ALU.add), reads=bvg, writes=[bcand, bv])
            for h0 in range(4):
                pair = (h0, h0 + 4)
                for k_, h in enumerate(pair):
                    K.op(DVE, lambda e, h=h: e.max(out=ctop[0:tp, h, 0:8], in_=cand[0:tp, h, :]), reads=[bcand],
                         writes=[bcth[h]])
                for k_, h in enumerate(pair):
                    K.op(DVE, lambda e, h=h: e.max_index(out=cidx[0:tp, h, 0:8], in_max=ctop[0:tp, h, 0:8],
                                                         in_values=cand[0:tp, h, :]), reads=[bcand, bcth[h]],
                         writes=[bcth[h]])
                for k_, h in enumerate(pair):
                    K.op(DVE, lambda e, h=h, k_=k_: e.match_replace(out=tmpB2[k_][0:tp, :], in_to_replace=ctop[0:tp, h, 0:8],
                                                                    in_values=cand[0:tp, h, :], imm_value=NEG),
                         reads=[bcand, bcth[h]], writes=[btB2[k_]])
                for k_, h in enumerate(pair):
                    K.op(DVE, lambda e, h=h, k_=k_: e.max(out=ctop[0:tp, h, 8:16], in_=tmpB2[k_][0:tp, :]),
                         reads=[btB2[k_]], writes=[bcth[h]])
                for k_, h in enumerate(pair):
                    K.op(DVE, lambda e, h=h, k_=k_: e.max_index(out=cidx[0:tp, h, 8:16], in_max=ctop[0:tp, h, 8:16],
                                                                in_values=tmpB2[k_][0:tp, :]), reads=[btB2[k_], bcth[h]],
                         writes=[bcth[h]])
            K.op(DVE, lambda e: e.tensor_copy(out=joinj[0:tp, 0:1], in_=ctop[0:tp, 0, 0:1]), reads=bcth, writes=[bct])
            K.op(DVE, lambda e: e.tensor_tensor(out=gate[0:tp, :, :], in0=ctop[0:tp, :, :],
                                                in1=ctop[0:tp, :, 0:1].to_broadcast([tp, 8, 16]), op=ALU.subtract),
                 reads=[bct], writes=[bgate])
            K.op(ACT, lambda e: e.activation(out=gate[0:tp, :, :], in_=gate[0:tp, :, :], func=AF.Exp), reads=[bgate],
                 writes=[bgate])
            K.op(DVE, lambda e: e.tensor_reduce(out=zs[0:tp, :], in_=gate[0:tp, :, :], axis=AX.X, op=ALU.add),
                 reads=[bgate], writes=[bgate])
            K.op(DVE, lambda e: e.reciprocal(out=zs[0:tp, :], in_=zs[0:tp, :]), reads=[bgate], writes=[bgate])
            K.op(DVE, lambda e: e.tensor_tensor(out=gate[0:tp, :, :], in0=gate[0:tp, :, :],
                                                in1=zs[0:tp, :].unsqueeze(2).to_broadcast([tp, 8, 16]), op=ALU.mult),
                 reads=[bgate], writes=[bgate])
            K.op(DVE, lambda e: e.tensor_single_scalar(out=iu[0:tp, :, :], in_=cidx[0:tp, :, :], scalar=4,
                                                       op=ALU.logical_shift_right), reads=[bct], writes=[bij])
            K.op(DVE, lambda e: e.tensor_single_scalar(out=ju[0:tp, :, :], in_=cidx[0:tp, :, :], scalar=15,
                                                       op=ALU.bitwise_and), reads=[bct], writes=[bij])
            K.op(DVE, lambda e: e.tensor_copy(out=i_f[0:tp, :, :], in_=iu[0:tp, :, :]), reads=[bij], writes=[bij])
            K.op(DVE, lambda e: e.tensor_copy(out=j_f[0:tp, :, :], in_=ju[0:tp, :, :]), reads=[bij], writes=[bij])
            for w_, pf in enumerate((i_f, j_f)):
                K.op(DVE, lambda e, pf=pf: e.tensor_tensor(
                    out=oh[0:tp], in0=pf[0:tp, :, :].unsqueeze(3).to_broadcast([tp, 8, 16, 16]),
                    in1=iota16[0:tp, :].unsqueeze(1).unsqueeze(1).to_broadcast([tp, 8, 16, 16]), op=ALU.is_equal),
                     reads=[bij, cst], writes=[boh])
                K.op(DVE, lambda e, w_=w_: e.tensor_tensor(
                    out=oh[0:tp], in0=oh[0:tp], in1=vv[:, :, w_, :].unsqueeze(2).to_broadcast([tp, 8, 16, 16]),
                    op=ALU.mult), reads=[boh, bv], writes=[boh])
                K.op(DVE, lambda e, w_=w_: e.tensor_reduce(out=vsel[0:tp, w_, :, :], in_=oh[0:tp], axis=AX.X,
                                                           op=ALU.add), reads=[boh], writes=[bvsel])
            s4 = s_sb[0:tp, :, :].rearrange("p (h two) k -> p h two k", two=2)
            for ih in range(4):
                E1, E2, bE1, bE2 = E1s[ectr % 2], E2s[ectr % 2], bE1s[ectr % 2], bE2s[ectr % 2]
                ectr += 1
                K.op(DVE, lambda e: e.tensor_tensor(
                    out=E1[0:tp], in0=s4[:, :, 0, ih * 32:(ih + 1) * 32].unsqueeze(2).to_broadcast([tp, 8, 16, 32]),
                    in1=vsel[0:tp, 0, :, :].unsqueeze(3).to_broadcast([tp, 8, 16, 32]), op=ALU.is_equal),
                     reads=[bs, bvsel], writes=[bE1])
                K.op(POOL, lambda e: e.tensor_tensor(
                    out=E1[0:tp], in0=E1[0:tp], in1=gate[0:tp, :, :].unsqueeze(3).to_broadcast([tp, 8, 16, 32]),
                    op=ALU.mult), reads=[bE1, bgate], writes=[bE1])
                K.op(DVE, lambda e: e.tensor_tensor(
                    out=E2[0:tp], in0=s4[:, :, 1, ih * 32:(ih + 1) * 32].unsqueeze(2).to_broadcast([tp, 8, 16, 32]),
                    in1=vsel[0:tp, 1, :, :].unsqueeze(3).to_broadcast([tp, 8, 16, 32]), op=ALU.is_equal),
                     reads=[bs, bvsel], writes=[bE2])
                for (Ex, bEx, XT, bXT, pbase) in ((E1, bE1, XT1, bXT1, 0), (E2, bE2, XT2, bXT2, 2)):
                    Ef = Ex[0:tp].rearrange("p h n i -> p (h n) i")
                    for i8 in range(4):
                        pb = pbase + (i8 % 2)
                        pst = psb[pb][:].bitcast(BF16)
                        for ii in range(8):
                            il = i8 * 8 + ii
                            K.op(PE, lambda e, il=il, ii=ii: e.transpose(out=pst[:, ii * 128:ii * 128 + tp],
                                                                         in_=Ef[:, :, il], identity=ident[0:tp, 0:tp]),
                                 reads=[bEx, cst], writes=[pbuf[pb]], signal=(ii == 7))
                        i1_0 = ih * 32 + i8 * 8
                        K.op(ACT, lambda e: e.activation(
                            out=XT[:, 0:tp, i1_0:i1_0 + 8],
                            in_=pst[:, :].rearrange("p (i t) -> p t i", i=8)[:, 0:tp, :], func=AF.Copy),
                             reads=[pbuf[pb]], writes=[bXT])
            for t4 in range(tp // 4):
                pb = 4 + (t4 % 4)
                psg = psb[pb][:, :].rearrange("p (i t) -> p i t", t=4)
                for tt in range(4):
                    t = t4 * 4 + tt
                    K.op(PE, lambda e, t=t, tt=tt: e.matmul(psg[:, :, tt], lhsT=XT1[:, t, :],
                                                            rhs=XT2[:, t, :], start=True, stop=True),
                         reads=[bXT1, bXT2], writes=[pbuf[pb]], signal=(tt == 3))
                K.op(ACT, lambda e: e.activation(out=Gsb[:, :, t4 * 4:t4 * 4 + 4], in_=psg[:, :, :], func=AF.Copy),
                     reads=[pbuf[pb]], writes=[bGsb])
            for q4 in range(4):
                K.dma(POOL,
                      Gs_d[tb, q4 * 32:(q4 + 1) * 32, :, :].rearrange("i2 i1 t -> i1 i2 t"),
                      Gsb[:, q4 * 32:(q4 + 1) * 32, :], reads=[bGsb], writes=[dbufs["Gs"]])
        K.barrier()
    if stop_after <= 5.5:
        K.finish()
        return nc


    eu_v = exp_u.rearrange("(a b) d -> b a d", b=128)
    with ExitStack() as st:
        def sb(name, shape, dt=F32):
            return st.enter_context(nc.sbuf_tensor(name, list(shape), dt))
        h2T = sb("h2Tb", [128, 16, NOWN], BF16)
        bh2T = Buf()
        for c in range(16):
            K.dma(SP if c % 2 == 0 else POOL, h2T[:, c, :], H2_d[c, :, :], reads=[dbufs["H2"]], writes=[bh2T])
        uf = [sb(f"uf{i}", [128, D]) for i in range(3)]
        ub = [sb(f"ub{i}", [128, D], BF16) for i in range(2)]
        UT = [sb(f"UT{i}", [128, 16, 128], BF16) for i in range(2)]
        gt = [sb(f"gt{i}", [128, NOWN], BF16) for i in range(2)]
        wt = [sb(f"wt{i}", [128, NOWN], BF16) for i in range(2)]
        gl = [sb(f"gl{i}", [128, 512], BF16) for i in range(3)]
        buf_ = [Buf() for _ in range(3)]
        bub = [Buf(), Buf()]
        bUT = [Buf(), Buf()]
        bgt = [Buf(), Buf()]
        bwt = [Buf(), Buf()]
        bgl = [Buf() for _ in range(3)]

        def p6_load(i2):
            K.dma(SP, uf[i2 % 3][:], eu_v[i2], writes=[buf_[i2 % 3]])

        def p6_prep(i2):
            k = i2 % 2
            u3 = i2 % 3
            K.dma(SP, gt[k][:, 0:2048].rearrange("p (b t) -> p b t", b=16),
                  Gs_d[0:16, i2, :, :].rearrange("b i t -> i b t"), reads=[dbufs["Gs"]], writes=[bgt[k]])
            K.dma(SP, gt[k][:, 2048:2112], Gs_d[16, i2, :, 0:64], reads=[dbufs["Gs"]], writes=[bgt[k]])
            K.op(DVE, lambda e: e.tensor_copy(out=ub[k][:], in_=uf[u3][:]), reads=[buf_[u3]], writes=[bub[k]])
            for half in range(2):
                pb = 6 + half
                pst = psb[pb][:].bitcast(BF16)
                for cc in range(8):
                    c = half * 8 + cc
                    K.op(PE, lambda e, c=c, cc=cc: e.transpose(out=pst[:, cc * 128:(cc + 1) * 128],
                                                               in_=ub[k][:, c * 128:(c + 1) * 128], identity=ident[:]),
                         reads=[bub[k], cst], writes=[pbuf[pb]], signal=(cc == 7))
                if half == 0:
                    K.op(ACT, lambda e: e.activation(out=UT[k][:, 0:8, :].rearrange("p c i -> p (c i)"), in_=pst[:, :],
                                                     func=AF.Copy), reads=[pbuf[pb]], writes=[bUT[k]])
                else:
                    K.op(DVE, lambda e: e.tensor_copy(out=UT[k][:, 8:16, :].rearrange("p c i -> p (c i)"), in_=pst[:, :]),
                         reads=[pbuf[pb]], writes=[bUT[k]])

        gi = 0
        p6_load(0)
        p6_load(1)
        p6_prep(0)
        for i2 in range(128):
            k = i2 % 2
            if i2 + 2 < 128:
                p6_load(i2 + 2)
            if i2 + 1 < 128:
                p6_prep(i2 + 1)
            for ti_, (n0, n) in enumerate(own_tiles):
                pb = gi % 6
                g3 = gi % 3
                gi += 1
                for c in range(16):
                    K.op(PE, lambda e, c=c: e.matmul(psb[pb][:, 0:n], lhsT=UT[k][:, c, :], rhs=h2T[:, c, n0:n0 + n],
                                                     start=(c == 0), stop=(c == 15)), reads=[bUT[k], bh2T],
                         writes=[pbuf[pb]], signal=(c == 15))
                K.op(ACT, lambda e: e.activation(out=gl[g3][:, 0:n], in_=psb[pb][:, 0:n], func=AF.Gelu),
                     reads=[pbuf[pb]], writes=[bgl[g3]])
                K.op(POOL, lambda e: e.tensor_tensor(out=wt[k][:, n0:n0 + n], in0=gl[g3][:, 0:n], in1=gt[k][:, n0:n0 + n],
                                                     op=ALU.mult), reads=[bgl[g3], bgt[k]], writes=[bwt[k]])
            K.dma(POOL, Ws_d[i2, :, :], wt[k][:], reads=[bwt[k]], writes=[dbufs["Ws"]])
        K.barrier()
    if stop_after <= 6:
        K.finish()
        return nc

    ev_v = exp_v.rearrange("(a b) d -> b a d", b=128)
    EG = 4
    with ExitStack() as st:
        def sb(name, shape, dt=F32):
            return st.enter_context(nc.sbuf_tensor(name, list(shape), dt))
        acc = sb("acc", [128, 17, 1024])
        bacc = Buf()
        wts = [sb(f"wts{i}", [128, EG, NOWN], BF16) for i in range(2)]
        bwts = [Buf(), Buf()]
        vb = [sb(f"vb{i}", [128, EG, 1024], BF16) for i in range(2)]
        bvb = [Buf(), Buf()]
        vf = [sb(f"vf7{i}", [128, 1024]) for i in range(3)]
        bvf = [Buf() for _ in range(3)]
        G2 = sb("G2", [128, 1024])
        bG2 = Buf()
        x1t = [sb(f"x1t{i}", [128, 1024]) for i in range(2)]
        bx1t = [Buf(), Buf()]
        vi = 0
        for dh in range(2):
            for eg in range(128 // EG):
                k = eg % 2
                K.dma(SP, wts[k][:], Ws_d[eg * EG:(eg + 1) * EG, :, :].rearrange("e p t -> p e t"),
                      reads=[dbufs["Ws"]], writes=[bwts[k]])
                for e_ in range(EG):
                    i2 = eg * EG + e_
                    v3 = vi % 3
                    vi += 1
                    K.dma(SP, vf[v3][:], ev_v[i2, :, dh * 1024:(dh + 1) * 1024], writes=[bvf[v3]])
                    K.op(ACT, lambda e, v3=v3, e_=e_: e.activation(out=vb[k][:, e_, :], in_=vf[v3][:], func=AF.Copy),
                         reads=[bvf[v3]], writes=[bvb[k]])
                for tb, (t0, tp) in enumerate(own_blocks):
                    pbs = (2 * (tb % 4), 2 * (tb % 4) + 1)
                    for e_ in range(EG):
                        for dt_ in range(2):
                            K.op(PE, lambda e, e_=e_, dt_=dt_: e.matmul(psb[pbs[dt_]][0:tp, :], lhsT=wts[k][:, e_, t0:t0 + tp],
                                                                        rhs=vb[k][:, e_, dt_ * 512:(dt_ + 1) * 512],
                                                                        start=(e_ == 0), stop=(e_ == EG - 1)),
                                 reads=[bwts[k], bvb[k]], writes=[pbuf[pbs[dt_]]], signal=(e_ == EG - 1))
                    for dt_ in range(2):
                        dst = acc[0:tp, tb, dt_ * 512:(dt_ + 1) * 512]
                        if eg == 0:
                            K.op(DVE, lambda e, dt_=dt_, dst=dst: e.tensor_copy(out=dst, in_=psb[pbs[dt_]][0:tp, :]),
                                 reads=[pbuf[pbs[dt_]]], writes=[bacc])
                        else:
                            K.op(DVE, lambda e, dt_=dt_, dst=dst: e.tensor_tensor(out=dst, in0=psb[pbs[dt_]][0:tp, :],
                                                                                  in1=dst, op=ALU.add),
                                 reads=[pbuf[pbs[dt_]], bacc], writes=[bacc])
            K.dma(SP, G2[:], gS_d[1, 0, dh * 1024:(dh + 1) * 1024].unsqueeze(0).to_broadcast([128, 1024]),
                  reads=[dbufs["gS"]], writes=[bG2])
            for tb, (t0, tp) in enumerate(own_blocks):
                if tb == 16:
                    for s_ in range(4):
                        K.dma(SP, G2[16 * s_:16 * s_ + 16, :],
                              gS_d[1, 1 + s_, dh * 1024:(dh + 1) * 1024].unsqueeze(0).to_broadcast([16, 1024]),
                              reads=[dbufs["gS"]], writes=[bG2])
                xt, bxt = x1t[tb % 2], bx1t[tb % 2]
                K.dma(SP, xt[0:tp, :], X1_d[t0:t0 + tp, dh * 1024:(dh + 1) * 1024], reads=[dbufs["X1"]], writes=[bxt])
                K.op(DVE, lambda e: e.tensor_tensor(out=acc[0:tp, tb, :], in0=acc[0:tp, tb, :], in1=G2[0:tp, :],
                                                    op=ALU.mult), reads=[bacc, bG2], writes=[bacc])
                K.op(POOL, lambda e: e.tensor_tensor(out=xt[0:tp, :], in0=xt[0:tp, :], in1=acc[0:tp, tb, :], op=ALU.add),
                     reads=[bacc, bxt], writes=[bxt])
                K.dma(POOL, X1_d[t0:t0 + tp, dh * 1024:(dh + 1) * 1024], xt[0:tp, :], reads=[bxt], writes=[dbufs["X1"]])
        K.barrier()
    if stop_after <= 7:
        K.finish()
        return nc

    with ExitStack() as st:
        def sb(name, shape, dt=F32):
            return st.enter_context(nc.sbuf_tensor(name, list(shape), dt))
        FG = sb("FG", [128, D])
        bFG = Buf()
        K.dma(SP, FG[:], final_g.unsqueeze(0).to_broadcast([128, D]), writes=[bFG])
        xf = [sb(f"xf{i}", [128, D]) for i in range(3)]
        bxf = [Buf() for _ in range(3)]
        yf = [sb(f"yf{i}", [128, D]) for i in range(2)]
        byf = [Buf(), Buf()]
        junk = sb("junk8", [128, D], BF16)
        bjunk = Buf()
        ss8 = [sb(f"ss8{i}", [128, 2]) for i in range(2)]
        bss8 = [Buf(), Buf()]
        for tb, (t0, tp) in enumerate(own_blocks):
            xt, bxt = xf[tb % 3], bxf[tb % 3]
            yt, byt = yf[tb % 2], byf[tb % 2]
            ss, bss = ss8[tb % 2], bss8[tb % 2]
            K.dma(SP, xt[0:tp, :], X1_d[t0:t0 + tp, :], reads=[dbufs["X1"]], writes=[bxt])
            K.op(ACT, lambda e: e.activation(out=junk[0:tp, :], in_=xt[0:tp, :], func=AF.Square,
                                             accum_out=ss[0:tp, 0:1]), reads=[bxt], writes=[bss, bjunk])
            K.op(ACT, lambda e: e.activation(out=ss[0:tp, 1:2], in_=ss[0:tp, 0:1], func=AF.Ln, scale=1.0 / D,
                                             bias=eps1[0:tp, :]), reads=[bss, cst], writes=[bss])
            K.op(ACT, lambda e: e.activation(out=ss[0:tp, 1:2], in_=ss[0:tp, 1:2], func=AF.Exp, scale=-0.5),
                 reads=[bss], writes=[bss])
            K.op(DVE, lambda e: e.scalar_tensor_tensor(out=yt[0:tp, :], in0=xt[0:tp, :], scalar=ss[0:tp, 1:2],
                                                       in1=FG[0:tp, :], op0=ALU.mult, op1=ALU.mult),
                 reads=[bxt, bss, bFG], writes=[byt])
            K.dma(POOL, y_out[t0:t0 + tp, :], yt[0:tp, :], reads=[byt], is_output=True)
        K.barrier()

    K.finish()
    return nc


_NC_CACHE = {}


def _get_nc(debug=False, stop_after=99):
    key = (debug, stop_after)
    if key not in _NC_CACHE:
        _NC_CACHE[key] = build(debug=debug, stop_after=stop_after)
    return _NC_CACHE[key]


def make_in_maps(inputs):
    f = lambda k: np.ascontiguousarray(np.asarray(inputs[k], dtype=np.float32))
    xp, xs = f("x_prompt"), f("x_sample")
    cp, cs = f("c_prompt"), f("c_sample")
    ck, cv, stc = f("cache_k")[0], f("cache_v")[0], f("state_conv")[0]
    shared = {
        "norm1_g": f("norm1_g")[0], "norm2_g": f("norm2_g")[0], "final_g": f("final_norm_g"),
        "w_ada": f("w_ada")[0], "b_ada": f("b_ada")[0], "w_in": f("w_in")[0], "conv_w": f("conv_w")[0],
        "w_attn_proj": f("w_attn_proj")[0], "w_conv_out": f("w_conv_out")[0], "w_o": f("w_o")[0],
        "w_query": f("w_query")[0], "sub_keys": f("sub_keys")[0].reshape(16, 128, 128),
        "expert_u": f("expert_u")[0], "expert_v": f("expert_v")[0],
    }
    in_maps = []
    for c in range(8):
        b, half = c // 2, c % 2
        own = xp[b, half * 2048:(half + 1) * 2048]
        ctx = xp[b, 0:2048] if half == 1 else np.zeros((2048, D), np.float32)
        xall = np.concatenate([ctx, own, xs[4 * c:4 * c + 4].reshape(64, D)], axis=0)
        cvec = np.concatenate([cp[b:b + 1], cs[4 * c:4 * c + 4]], axis=0)
        flags = np.zeros((128, 2), np.float32)
        flags[:, 0] = 0.0 if half == 1 else -30000.0
        flags[:, 1] = 1.0 if half == 1 else 0.0
        m = dict(shared)
        m.update({
            "xall": np.ascontiguousarray(xall), "cvec": np.ascontiguousarray(cvec), "flags": flags,
            "cache_k": np.ascontiguousarray(ck[4 * c:4 * c + 4].reshape(4, 4096, 1024)),
            "cache_v": np.ascontiguousarray(cv[4 * c:4 * c + 4].reshape(4, 4096, 1024)),
            "state_conv": np.ascontiguousarray(stc[4 * c:4 * c + 4]),
        })
        in_maps.append(m)
    return in_maps


def kernel(**inputs):
    nc = _get_nc()
    in_maps = make_in_maps(inputs)
    in_maps = [{k: v for k, v in m.items() if k in LAST_INPUT_NAMES} for m in in_maps]
    res = run_bass_kernel_spmd(nc, in_maps, core_ids=list(range(8)))
    R = res.results
    y_prompt = np.zeros((4, 4096, D), np.float32)
    y_sample = np.zeros((32, 16, D), np.float32)
    nkp = np.zeros((1, 4, 4096, 8, 128), np.float32)
    nvp = np.zeros((1, 4, 4096, 8, 128), np.float32)
    ncp = np.zeros((1, 4, 2, 1024), np.float32)
    nks = np.zeros((1, 32, 16, 8, 128), np.float32)
    nvs = np.zeros((1, 32, 16, 8, 128), np.float32)
    ncs = np.zeros((1, 32, 2, 1024), np.float32)
    for c in range(8):
        b, half = c // 2, c % 2
        r = R[c]
        sl = slice(half * 2048, (half + 1) * 2048)
        y_prompt[b, sl] = r["y"][:2048]
        y_sample[4 * c:4 * c + 4] = r["y"][2048:].reshape(4, 16, D)
        nkp[0, b, sl] = r["nk"][:2048].reshape(2048, 8, 128)
        nvp[0, b, sl] = r["nv"][:2048].reshape(2048, 8, 128)
        nks[0, 4 * c:4 * c + 4] = r["nk"][2048:].reshape(4, 16, 8, 128)
        nvs[0, 4 * c:4 * c + 4] = r["nv"][2048:].reshape(4, 16, 8, 128)
        if half == 1:
            ncp[0, b] = r["nconv"][0]
        ncs[0, 4 * c:4 * c + 4] = r["nconv"][1:]
    return (y_prompt, y_sample, nkp, nvp, ncp, nks, nvs, ncs)
```

```python
from contextlib import ExitStack
import numpy as np
import concourse.bass as bass
import concourse.mybir as mybir
from concourse.bass_utils import run_bass_kernel_spmd

F32 = mybir.dt.float32
BF16 = mybir.dt.bfloat16
U32 = mybir.dt.uint32
I32 = mybir.dt.int32
AF = mybir.ActivationFunctionType
ALU = mybir.AluOpType
AX = mybir.AxisListType

D = 2048
NCTX = 2048
NOWN = 2112
NTOK = NCTX + NOWN
NSEQ = 5
INC = 10240
NEXP = 16384
SEM_LIMIT = 20000
NEG = -1.0e30


class Buf:
    __slots__ = ("name", "w", "r", "excl")

    def __init__(self, name="", excl=False):
        self.name = name
        self.w = None
        self.r = {}
        self.excl = excl


class Eng:
    def __init__(self, K, name, h, same_sync):
        self.K = K
        self.name = name
        self.h = h
        self.same_sync = same_sync
        self.sem = K.new_sem(name)
        self.count = 0
        self.known = {}
        self.nsem = 0
        self.dpool = []
        self.dnext = 0

    def new_counter(self):
        self.nsem += 1
        self.sem = self.K.new_sem(f"{self.name}{self.nsem}")
        self.count = 0


class Kern:
    def __init__(self, nc, ndma_sems=10):
        self.nc = nc
        self._nsem = 0
        self.pe = Eng(self, "pe", nc.tensor, False)
        self.act = Eng(self, "act", nc.scalar, True)
        self.dve = Eng(self, "dve", nc.vector, True)
        self.pool = Eng(self, "pool", nc.gpsimd, True)
        self.sp = Eng(self, "sp", nc.sync, False)
        self.engs = [self.pe, self.act, self.dve, self.pool, self.sp]
        for e in (self.sp, self.act, self.pool):
            for i in range(ndma_sems):
                e.dpool.append([self.new_sem(f"d{e.name}{i}"), 0])
        self.out_events = []

    def new_sem(self, name):
        self._nsem += 1
        return self.nc.alloc_semaphore(f"s_{name}_{self._nsem}")

    def _wait(self, E, ev):
        if ev is None:
            return
        sem, val = ev
        if E.known.get(id(sem), (None, 0))[1] >= val:
            return
        E.h.wait_ge(sem, val)
        E.known[id(sem)] = (sem, val)

    def _deps(self, E, reads, writes):
        evs = {}

        def add(ev):
            if ev is None:
                return
            k = id(ev[0])
            if k not in evs or evs[k][1] < ev[1]:
                evs[k] = ev
        for b in reads:
            add(b.w)
        for b in writes:
            add(b.w)
            for ev in b.r.values():
                add(ev)
        for ev in evs.values():
            if (not E.same_sync) and ev[0] is E.sem:
                continue
            self._wait(E, ev)

    def _record(self, ev, reads, writes):
        k = id(ev[0])
        for b in reads:
            if k not in b.r or b.r[k][1] < ev[1]:
                b.r[k] = ev
        for b in writes:
            b.w = ev
            b.r = {}

    def op(self, E, fn, reads=(), writes=(), signal=True):
        ex = [b for b in reads if b.excl]
        if ex:
            writes = list(writes) + ex
        self._deps(E, reads, writes)
        ins = fn(E.h)
        if signal:
            if E.count >= SEM_LIMIT:
                E.new_counter()
            E.count += 1
            ins.then_inc(E.sem, 1)
            ev = (E.sem, E.count)
            self._record(ev, reads, writes)
            return ev
        return None

    def dma(self, E, out, in_, reads=(), writes=(), is_output=False, **kw):
        self._deps(E, reads, writes)
        slot = E.dpool[E.dnext % len(E.dpool)]
        E.dnext += 1
        if slot[1] > 0:
            self._wait(E, (slot[0], slot[1]))
        if slot[1] >= SEM_LIMIT:
            slot[0] = self.new_sem("dx")
            slot[1] = 0
        ins = E.h.dma_start(out=out, in_=in_, **kw)
        slot[1] += 16
        ins.then_inc(slot[0], 16)
        ev = (slot[0], slot[1])
        self._record(ev, reads, writes)
        if is_output:
            self.out_events.append(ev)
        return ev

    def barrier(self):
        evs = []
        for e in self.engs:
            if e.count > 0:
                evs.append((e.sem, e.count))
        for e in (self.sp, self.act, self.pool):
            for slot in e.dpool:
                if slot[1] > 0:
                    evs.append((slot[0], slot[1]))
        for E in self.engs:
            for ev in evs:
                if ev[0] is E.sem:
                    continue
                self._wait(E, ev)

    def finish(self):
        self.barrier()


LAST_INPUT_NAMES = set()


def build(debug=False, stop_after=99):
    nc = bass.Bass("TRN2", target_bir_lowering=False)
    K = Kern(nc)
    PE, ACT, DVE, POOL, SP = K.pe, K.act, K.dve, K.pool, K.sp

    LAST_INPUT_NAMES.clear()

    def din(name, shape, dt=F32):
        LAST_INPUT_NAMES.add(name)
        return nc.dram_tensor(name, list(shape), dt, kind="ExternalInput").ap()

    def dout(name, shape, dt=F32):
        return nc.dram_tensor(name, list(shape), dt, kind="ExternalOutput").ap()

    def dscr(name, shape, dt):
        kind = "ExternalOutput" if (debug and name in debug) else "Internal"
        return nc.dram_tensor(name, list(shape), dt, kind=kind).ap()

    xall = din("xall", [NTOK, D])
    cvec = din("cvec", [NSEQ, D])
    flags = din("flags", [128, 2])
    cache_k = din("cache_k", [4, 4096, 1024]) if stop_after > 2.5 else None
    cache_v = din("cache_v", [4, 4096, 1024]) if stop_after > 2.5 else None
    state_conv = din("state_conv", [4, 2, 1024])
    norm1_g = din("norm1_g", [D])
    norm2_g = din("norm2_g", [D])
    final_g = din("final_g", [D])
    w_ada = din("w_ada", [D, 6 * D])
    b_ada = din("b_ada", [6 * D])
    w_in = din("w_in", [D, INC])
    conv_w = din("conv_w", [3, 1024])
    w_ap = din("w_attn_proj", [1024, D]) if stop_after >= 4 else None
    w_co = din("w_conv_out", [1024, D]) if stop_after >= 4 else None
    w_o = din("w_o", [D, D]) if stop_after >= 4 else None
    w_q = din("w_query", [D, D]) if stop_after >= 5 else None
    sub_keys = din("sub_keys", [16, 128, 128]) if stop_after >= 5 else None
    exp_u = din("expert_u", [NEXP, D]) if stop_after >= 6 else None
    exp_v = din("expert_v", [NEXP, D]) if stop_after >= 7 else None
    y_out = dout("y", [NOWN, D])
    nk_out = dout("nk", [NOWN, 1024])
    nv_out = dout("nv", [NOWN, 1024])
    nconv_out = dout("nconv", [NSEQ, 2, 1024])
    gS_d = dscr("gS_d", [2, NSEQ, D], F32)
    hT_d = dscr("hT_d", [16, 128, NTOK], BF16)
    KT_d = dscr("KT_d", [8, 128, NTOK], BF16)
    V_d = dscr("V_d", [NTOK, 1024], BF16)
    QT_d = dscr("QT_d", [8, 128, NOWN], BF16)
    GC_d = dscr("GC_d", [8, 128, NOWN], BF16)
    SG_d = dscr("SG_d", [2, 16, 128, NOWN], BF16)
    OT_d = dscr("OT_d", [8, 128, NOWN], BF16)
    X1_d = dscr("X1_d", [NOWN, D], F32)
    H2_d = dscr("H2_d", [16, 128, NOWN], BF16)
    MG_d = dscr("MG_d", [16, 128, NOWN], BF16)
    S_d = dscr("S_d", [17, 128, 2048], F32)
    Gs_d = dscr("Gs_d", [17, 128, 128, 128], BF16)
    Ws_d = dscr("Ws_d", [128, 128, NOWN], BF16)
    dbufs = {n: Buf(n) for n in ["gS", "hT", "KT", "V", "QT", "GC", "SG", "OT", "X1", "H2", "S", "Gs", "Ws", "MG"]}

    psb = [nc.alloc_psum_tensor(f"psb{i}", [128, 512], F32) for i in range(8)]
    pbuf = [Buf(f"ps{i}", excl=True) for i in range(8)]

    def sbp(name, shape, dt=F32):
        return nc.alloc_sbuf_tensor(name, list(shape), dt)

    ident = sbp("ident", [128, 128], BF16)
    nti = sbp("nti", [128, 128], BF16)
    neg1 = sbp("neg1", [128, 128], BF16)
    maskd = sbp("maskd", [128, 4, 512], BF16)
    masks = sbp("masks", [16, 8, 16], BF16)
    flg = sbp("flg", [128, 2], F32)
    zero1 = sbp("zero1", [128, 1], F32)
    eps1 = sbp("eps1", [128, 1], F32)
    iota16 = sbp("iota16", [128, 16], F32)
    A1 = sbp("A1", [128, 16, NSEQ], F32)
    B1 = sbp("B1", [128, 16, NSEQ], F32)
    A2 = sbp("A2", [128, 16, NSEQ], F32)
    B2 = sbp("B2", [128, 16, NSEQ], F32)
    fgT = sbp("fgT", [128, 16], F32)
    cw = sbp("cw", [128, 3, 8], F32)
    uprev = sbp("uprev", [128, 8, 2], F32)
    cst = Buf("consts")
    bmod = Buf("mod")
    buprev = Buf("uprev")

    nc_allow = nc.allow_non_contiguous_dma(reason="small strided parameter loads")
    nc_allow.__enter__()

    tmpf = sbp("tmpf", [128, 512], F32)
    K.op(POOL, lambda e: e.memset(tmpf[:, 0:128], 0.0), writes=[cst])
    K.op(POOL, lambda e: e.affine_select(out=tmpf[:, 0:128], in_=tmpf[:, 0:128], pattern=[[-1, 128]],
                                         compare_op=ALU.not_equal, fill=1.0, base=0, channel_multiplier=1),
         reads=[cst], writes=[cst])
    K.op(POOL, lambda e: e.tensor_copy(out=ident[:], in_=tmpf[:, 0:128]), reads=[cst], writes=[cst])
    K.op(POOL, lambda e: e.memset(tmpf[:, 0:128], -1.0), reads=[cst], writes=[cst])
    K.op(POOL, lambda e: e.tensor_copy(out=neg1[:], in_=tmpf[:, 0:128]), reads=[cst], writes=[cst])
    K.op(POOL, lambda e: e.affine_select(out=tmpf[:, 0:128], in_=tmpf[:, 0:128], pattern=[[-1, 128]],
                                         compare_op=ALU.is_ge, fill=0.0, base=0, channel_multiplier=1),
         reads=[cst], writes=[cst])
    K.op(POOL, lambda e: e.tensor_copy(out=nti[:], in_=tmpf[:, 0:128]), reads=[cst], writes=[cst])
    for m in range(4):
        K.op(POOL, lambda e: e.memset(tmpf[:], 1.0), reads=[cst], writes=[cst])
        K.op(POOL, lambda e, m=m: e.affine_select(out=tmpf[:], in_=tmpf[:], pattern=[[1, 512]],
                                                  compare_op=ALU.is_gt, fill=0.0, base=-128 * m,
                                                  channel_multiplier=-1), reads=[cst], writes=[cst])
        K.op(POOL, lambda e, m=m: e.tensor_copy(out=maskd[:, m, :], in_=tmpf[:]), reads=[cst], writes=[cst])
    K.op(POOL, lambda e: e.memset(tmpf[0:16, 0:128], 1.0), reads=[cst], writes=[cst])
    K.op(POOL, lambda e: e.affine_select(out=tmpf[0:16, 0:128].rearrange("p (h i) -> p h i", h=8),
                                         in_=tmpf[0:16, 0:128].rearrange("p (h i) -> p h i", h=8),
                                         pattern=[[0, 8], [1, 16]], compare_op=ALU.is_gt, fill=0.0, base=0,
                                         channel_multiplier=-1), reads=[cst], writes=[cst])
    K.op(POOL, lambda e: e.tensor_copy(out=masks[:], in_=tmpf[0:16, 0:128].rearrange("p (h i) -> p h i", h=8)),
         reads=[cst], writes=[cst])
    K.op(POOL, lambda e: e.memset(zero1[:], 0.0), reads=[cst], writes=[cst])
    K.op(POOL, lambda e: e.memset(eps1[:], 1e-6), reads=[cst], writes=[cst])
    K.op(POOL, lambda e: e.iota(iota16[:], pattern=[[1, 16]], base=0, channel_multiplier=0,
                                allow_small_or_imprecise_dtypes=True), reads=[cst], writes=[cst])
    K.dma(SP, flg[:], flags[:, :], writes=[cst])
    K.dma(SP, fgT[:], final_g.rearrange("(c p) -> p c", p=128), writes=[cst])
    for r in range(3):
        K.dma(SP, cw[:, r, :], conv_w[r].rearrange("(j p) -> p j", p=128), writes=[cst])

    with ExitStack() as st:
        def sb(name, shape, dt=F32):
            return st.enter_context(nc.sbuf_tensor(name, list(shape), dt))
        cT = sb("cT", [128, 16, NSEQ])
        bcT = Buf()
        bA = sb("bA", [128, 96])
        n1g = sb("n1g", [128, 16])
        n2g = sb("n2g", [128, 16])
        modT = sb("modT", [128, 6, 16, NSEQ])
        wbuf = [sb(f"wada{i}", [128, 16, 512]) for i in range(2)]
        bwbuf = [Buf(), Buf()]
        gtm = sb("gtm", [NSEQ, 2, D])
        brow = sb("brow", [NSEQ, 2, D])
        bg = Buf()
        for s_ in range(NSEQ):
            K.dma(SP, cT[:, :, s_], cvec[s_].rearrange("(c p) -> p c", p=128), writes=[bcT])
        K.dma(SP, bA[:], b_ada.rearrange("(b p) -> p b", p=128), writes=[bmod])
        K.dma(SP, n1g[:], norm1_g.rearrange("(c p) -> p c", p=128), writes=[bmod])
        K.dma(SP, n2g[:], norm2_g.rearrange("(c p) -> p c", p=128), writes=[bmod])
        for wi, which in enumerate((2, 5)):
            K.dma(SP, brow[:, wi, :], b_ada[which * D:(which + 1) * D].unsqueeze(0).to_broadcast([NSEQ, D]),
                  writes=[bg])
        K.op(ACT, lambda e: e.activation(out=cT[:], in_=cT[:], func=AF.Silu), reads=[bcT], writes=[bcT])
        cTb = sb("cTb", [128, 16, NSEQ], BF16)
        K.op(DVE, lambda e: e.tensor_copy(out=cTb[:], in_=cT[:]), reads=[bcT], writes=[bcT])
        wadab = [sb(f"wadab{i}", [128, 16, 512], BF16) for i in range(2)]
        bwadab = [Buf(), Buf()]
        gi = 0
        for which in range(6):
            for q4 in range(4):
                col0 = which * D + q4 * 512
                wb, bw = wbuf[gi % 2], bwbuf[gi % 2]
                K.dma(SP if gi % 2 == 0 else POOL, wb[:], w_ada[:, col0:col0 + 512].rearrange("(c p) n -> p c n", p=128),
                      writes=[bw])
                pi = gi % 2
                if which in (2, 5):
                    wi = 0 if which == 2 else 1
                    for c in range(16):
                        K.op(PE, lambda e, c=c: e.matmul(psb[pi][0:NSEQ, :], lhsT=cT[:, c, :], rhs=wb[:, c, :],
                                                         start=(c == 0), stop=(c == 15)),
                             reads=[bcT, bw], writes=[pbuf[pi]], signal=(c == 15))
                    K.op(DVE, lambda e: e.tensor_tensor(out=gtm[:, wi, q4 * 512:(q4 + 1) * 512], in0=psb[pi][0:NSEQ, :],
                                                        in1=brow[:, wi, q4 * 512:(q4 + 1) * 512], op=ALU.add),
                         reads=[pbuf[pi], bg], writes=[bg])
                else:
                    wbb, bwbb = wadab[gi % 2], bwadab[gi % 2]
                    K.op(DVE if gi % 2 == 0 else ACT,
                         (lambda e: e.tensor_copy(out=wbb[:], in_=wb[:])) if gi % 2 == 0 else
                         (lambda e: e.activation(out=wbb[:], in_=wb[:], func=AF.Copy)), reads=[bw], writes=[bwbb])
                    for j in range(4):
                        for c in range(16):
                            K.op(PE, lambda e, c=c, j=j: e.matmul(psb[pi][:, j * NSEQ:(j + 1) * NSEQ],
                                                                  lhsT=wbb[:, c, j * 128:(j + 1) * 128], rhs=cTb[:, c, :],
                                                                  start=(c == 0), stop=(c == 15)),
                                 reads=[bcT, bwbb], writes=[pbuf[pi]], signal=(c == 15 and j == 3))
                    blk0 = which * 16 + q4 * 4
                    K.op(DVE, lambda e: e.tensor_tensor(
                        out=modT[:, which, q4 * 4:(q4 + 1) * 4, :],
                        in0=psb[pi][:, 0:4 * NSEQ].rearrange("p (j s) -> p j s", j=4),
                        in1=bA[:, blk0:blk0 + 4].unsqueeze(2).to_broadcast([128, 4, NSEQ]), op=ALU.add),
                         reads=[pbuf[pi], bmod], writes=[bmod])
                gi += 1
        for (Aa, Bb, gg, isc, ish) in ((A1, B1, n1g, 1, 0), (A2, B2, n2g, 4, 3)):
            K.op(DVE, lambda e, Aa=Aa, isc=isc: e.tensor_scalar(out=Aa[:], in0=modT[:, isc, :, :], scalar1=1.0, scalar2=None,
                                                                op0=ALU.add), reads=[bmod], writes=[bmod])
            K.op(DVE, lambda e, Aa=Aa, gg=gg: e.tensor_tensor(out=Aa[:], in0=Aa[:],
                                                              in1=gg[:].unsqueeze(2).to_broadcast([128, 16, NSEQ]),
                                                              op=ALU.mult), reads=[bmod], writes=[bmod])
            K.op(DVE, lambda e, Bb=Bb, ish=ish: e.tensor_copy(out=Bb[:], in_=modT[:, ish, :, :]), reads=[bmod],
                 writes=[bmod])
        K.dma(SP, gS_d.rearrange("w s d -> s w d"), gtm[:], reads=[bg], writes=[dbufs["gS"]])
        K.barrier()
    if stop_after <= 0:
        K.finish()
        return nc

    def seq_groups(tb):
        if tb < 32:
            return [(0, 0, 128)]
        return [(1 + i, 16 * i, 16) for i in range(4)]

    class NormCtx:
        def __init__(self, st, tag):
            self.junk = st.enter_context(nc.sbuf_tensor(f"junk{tag}", [128, D], BF16))
            self.xn = [st.enter_context(nc.sbuf_tensor(f"xn{tag}{i}", [128, D], BF16)) for i in range(2)]
            self.bxn = [Buf(), Buf()]
            self.ss = [st.enter_context(nc.sbuf_tensor(f"ss{tag}{i}", [128, 2], F32)) for i in range(2)]
            self.bss = [Buf(), Buf()]
            self.bjunk = Buf()
            self.i = 0

    def emit_norm_T(ctx, xt, bxt, tp, groups, Aa, Bb, hT_tile, bhT, tcol0, pbanks):
        st_ = emit_norm_stats(ctx, xt, bxt, tp)
        emit_norm_tr(ctx, st_, tp, groups, Aa, Bb, hT_tile, bhT, tcol0, pbanks)

    def emit_norm_stats(ctx, xt, bxt, tp):
        i = ctx.i
        ctx.i += 1
        xn, bxn, ss, bss = ctx.xn[i % 2], ctx.bxn[i % 2], ctx.ss[i % 2], ctx.bss[i % 2]
        K.op(ACT, lambda e: e.activation(out=ctx.junk[0:tp, :], in_=xt[0:tp, :], func=AF.Square,
                                         accum_out=ss[0:tp, 0:1]), reads=[bxt], writes=[bss, ctx.bjunk])
        K.op(ACT, lambda e: e.activation(out=ss[0:tp, 1:2], in_=ss[0:tp, 0:1], func=AF.Ln, scale=1.0 / D,
                                         bias=eps1[0:tp, :]), reads=[bss, cst], writes=[bss])
        K.op(ACT, lambda e: e.activation(out=ss[0:tp, 1:2], in_=ss[0:tp, 1:2], func=AF.Exp, scale=-0.5),
             reads=[bss], writes=[bss])
        K.op(DVE, lambda e: e.tensor_scalar(out=xn[0:tp, :], in0=xt[0:tp, :], scalar1=ss[0:tp, 1:2], scalar2=None,
                                            op0=ALU.mult), reads=[bxt, bss], writes=[bxn])
        return (xn, bxn)

    def emit_norm_tr(ctx, st_, tp, groups, Aa, Bb, hT_tile, bhT, tcol0, pbanks):
        xn, bxn = st_
        for half in range(2):
            pb = pbanks[half]
            pst = psb[pb][:].bitcast(BF16)
            for cc in range(8):
                c = half * 8 + cc
                K.op(PE, lambda e, c=c, cc=cc: e.transpose(out=pst[:, cc * 128:cc * 128 + tp],
                                                           in_=xn[0:tp, c * 128:(c + 1) * 128],
                                                           identity=ident[0:tp, 0:tp]),
                     reads=[bxn, cst], writes=[pbuf[pb]], signal=(cc == 7))
            for cc in range(8):
                c = half * 8 + cc
                for (s, g0, gn) in groups:
                    src = pst[:, cc * 128 + g0:cc * 128 + g0 + gn]
                    dst = hT_tile[:, c, tcol0 + g0:tcol0 + g0 + gn]
                    if half == 0:
                        K.op(ACT, lambda e, src=src, dst=dst, c=c, s=s: e.activation(
                            out=dst, in_=src, func=AF.Identity, scale=Aa[:, c, s:s + 1], bias=Bb[:, c, s:s + 1]),
                             reads=[pbuf[pb], bmod], writes=[bhT])
                    else:
                        K.op(DVE, lambda e, src=src, dst=dst, c=c, s=s: e.tensor_scalar(
                            out=dst, in0=src, scalar1=Aa[:, c, s:s + 1], scalar2=Bb[:, c, s:s + 1],
                            op0=ALU.mult, op1=ALU.add), reads=[pbuf[pb], bmod], writes=[bhT])

    with ExitStack() as st:
        def sb(name, shape, dt=F32):
            return st.enter_context(nc.sbuf_tensor(name, list(shape), dt))
        nctx = NormCtx(st, "a")
        xts = [sb(f"xt{i}", [128, D]) for i in range(3)]
        bxts = [Buf() for _ in range(3)]
        hTt = [sb(f"hTt{i}", [128, 16, 512], BF16) for i in range(2)]
        bhTt = [Buf(), Buf()]
        def p1_stats(tb):
            tp = 128 if tb < 32 else 64
            xt, bxt = xts[tb % 3], bxts[tb % 3]
            K.dma(SP, xt[0:tp, :], xall[tb * 128:tb * 128 + tp, :], writes=[bxt])
            return emit_norm_stats(nctx, xt, bxt, tp)
        pend = p1_stats(0)
        for nt in range(9):
            t0 = nt * 512
            nblk = 4 if nt < 8 else 1
            ht, bht = hTt[nt % 2], bhTt[nt % 2]
            ncols = 0
            for bi in range(nblk):
                tb = nt * 4 + bi
                tp = 128 if tb < 32 else 64
                cur = pend
                if tb + 1 < 33:
                    pend = p1_stats(tb + 1)
                emit_norm_tr(nctx, cur, tp, seq_groups(tb), A1, B1, ht, bht, bi * 128, (0 + 2 * (tb % 2), 1 + 2 * (tb % 2)))
                ncols += tp
            K.dma(POOL, hT_d[:, :, t0:t0 + ncols].rearrange("c p t -> p c t"), ht[:, :, 0:ncols], reads=[bht],
                  writes=[dbufs["hT"]])
        K.barrier()
    if stop_after <= 1:
        K.finish()
        return nc


    bankctr = [0]

    def nextbank():
        b = bankctr[0] % 8
        bankctr[0] += 1
        return b

    def proj_pass(is_ctx):
        tok0 = 0 if is_ctx else NCTX
        ntok = NCTX if is_ctx else NOWN
        ntiles = [(i * 512, 512) for i in range(4)] + ([] if is_ctx else [(2048, 64)])
        tblocks = [(i * 128, 128) for i in range(16)] + ([] if is_ctx else [(2048, 64)])
        with ExitStack() as st:
            def sb(name, shape, dt=F32):
                return st.enter_context(nc.sbuf_tensor(name + ("c" if is_ctx else "o"), list(shape), dt))
            hT_sb = sb("hT_sb", [128, 16, ntok], BF16)
            bhT = [Buf() for _ in range(16)]
            for c in range(16):
                K.dma(SP if c % 2 == 0 else POOL, hT_sb[:, c, :], hT_d[c, :, tok0:tok0 + ntok], reads=[dbufs["hT"]],
                      writes=[bhT[c]])
            wst = [sb(f"wst{i}", [128, 16, 128]) for i in range(3)]
            wbf = [sb(f"wbf{i}", [128, 16, 128], BF16) for i in range(2)]
            bwst = [Buf() for _ in range(3)]
            bwbf = [Buf(), Buf()]
            cnt = [0]
            if is_ctx:
                fm_cols = [1024 + h * 128 for h in range(8)]
                for j in range(8):
                    fm_cols += [3072 + j * 128, 5120 + j * 128]
            else:
                fm_cols = [1024 + h * 128 for h in range(8)] + [h * 128 for h in range(8)]
                for j in range(8):
                    fm_cols += [3072 + j * 128, 5120 + j * 128, 4096 + j * 128]
                fm_cols += [6144 + j * 128 for j in range(16)] + [8192 + j * 128 for j in range(16)]
            loaded = [0]
            casted = [0]

            def fm_load_upto(k):
                while loaded[0] <= k and loaded[0] < len(fm_cols):
                    i = loaded[0]
                    c0 = fm_cols[i]
                    K.dma(SP, wst[i % 3][:], w_in[:, c0:c0 + 128].rearrange("(c p) n -> p c n", p=128),
                          writes=[bwst[i % 3]])
                    loaded[0] += 1

            def fm_cast_upto(k):
                while casted[0] <= k and casted[0] < len(fm_cols):
                    i = casted[0]
                    fm_load_upto(i)
                    K.op(DVE, lambda e, i=i: e.tensor_copy(out=wbf[i % 2][:], in_=wst[i % 3][:]), reads=[bwst[i % 3]],
                         writes=[bwbf[i % 2]])
                    casted[0] += 1

            def fm_block(col0, tiles, evac):
                i = cnt[0]
                cnt[0] += 1
                assert fm_cols[i] == col0, (i, fm_cols[i], col0)
                fm_cast_upto(i)
                fm_load_upto(i + 2)
                wb, bwb = wbf[i % 2], bwbf[i % 2]
                first_tile = True
                for (n0, n) in tiles:
                    pb = nextbank()
                    for c in range(16):
                        K.op(PE, lambda e, c=c: e.matmul(psb[pb][:, 0:n], lhsT=wb[:, c, :], rhs=hT_sb[:, c, n0:n0 + n],
                                                         start=(c == 0), stop=(c == 15)),
                             reads=[bwb, bhT[c]], writes=[pbuf[pb]], signal=(c == 15))
                    evac(pb, n0, n)
                    if first_tile:
                        first_tile = False
                        fm_cast_upto(i + 1)

            with ExitStack() as st2:
                def sb2(name, shape, dt=F32):
                    return st2.enter_context(nc.sbuf_tensor(name + ("c" if is_ctx else "o"), list(shape), dt))
                obuf = [sb2(f"obuf{i}", [128, ntok], BF16) for i in range(4)]
                bobuf = [Buf() for _ in range(4)]
                octr = [0]

                def simple_block(col0, fn, dst_ap, dbuf):
                    oi = octr[0] % 4
                    octr[0] += 1
                    ob, bob = obuf[oi], bobuf[oi]

                    def evac(pb, n0, n):
                        fn(pb, n0, n, ob, bob)
                    fm_block(col0, ntiles, evac)
                    K.dma(POOL, dst_ap, ob[:, 0:ntok], reads=[bob], writes=[dbuf])

                def ev_copy_dve(pb, n0, n, ob, bob):
                    K.op(DVE, lambda e: e.tensor_copy(out=ob[:, n0:n0 + n], in_=psb[pb][:, 0:n]), reads=[pbuf[pb]],
                         writes=[bob])

                def ev_scale_act(pb, n0, n, ob, bob):
                    K.op(ACT, lambda e: e.activation(out=ob[:, n0:n0 + n], in_=psb[pb][:, 0:n], func=AF.Copy,
                                                     scale=float(128 ** -0.5)), reads=[pbuf[pb]], writes=[bob])

                def ev_sigmoid(pb, n0, n, ob, bob):
                    K.op(ACT, lambda e: e.activation(out=ob[:, n0:n0 + n], in_=psb[pb][:, 0:n], func=AF.Sigmoid),
                         reads=[pbuf[pb]], writes=[bob])

                import os
                CUT = int(os.environ.get("DBG_CUT", "99"))
                for h in range(8 if CUT > 0 else 1):
                    simple_block(1024 + h * 128, ev_copy_dve, KT_d[h, :, tok0:tok0 + ntok], dbufs["KT"])
                if CUT <= 1:
                    K.barrier()
                    return
                if is_ctx:
                    HC2 = sb2("HC2", [128, 8, 2])
                    bHC2 = Buf()
                    for j in range(8):
                        def ev_hc2(pb, n0, n, j=j):
                            K.op(DVE, lambda e: e.tensor_copy(out=HC2[:, j, :], in_=psb[pb][:, 0:2]), reads=[pbuf[pb]],
                                 writes=[bHC2])
                        fm_block(3072 + j * 128, [(2046, 2)], ev_hc2)

                        def ev_gc2(pb, n0, n, j=j):
                            K.op(DVE, lambda e: e.scalar_tensor_tensor(out=uprev[:, j, :], in0=psb[pb][:, 0:2],
                                                                       scalar=flg[:, 1:2], in1=HC2[:, j, :],
                                                                       op0=ALU.mult, op1=ALU.mult),
                                 reads=[pbuf[pb], bHC2, cst], writes=[buprev])
                        fm_block(5120 + j * 128, [(2046, 2)], ev_gc2)
                else:
                    for h in range(8):
                        simple_block(h * 128, ev_scale_act, QT_d[h, :, :], dbufs["QT"])
                    if CUT <= 2:
                        K.barrier()
                        return
                    HCt = [sb2(f"HCt{i}", [128, NOWN], BF16) for i in range(2)]
                    bHCt = [Buf(), Buf()]
                    Uf = [sb2(f"Uf{i}", [128, 2122]) for i in range(2)]
                    bUf = [Buf(), Buf()]
                    CV = [sb2(f"CV{i}", [128, NOWN]) for i in range(2)]
                    bCV = [Buf(), Buf()]
                    segs = [(0, 0, 2048)] + [(2050 + 18 * s_, 2048 + 16 * s_, 16) for s_ in range(4)]
                    for j in range(8):
                        hct, bhct, uf, buf_, cv, bcv = HCt[j % 2], bHCt[j % 2], Uf[j % 2], bUf[j % 2], CV[j % 2], bCV[j % 2]

                        def ev_hc(pb, n0, n):
                            K.op(ACT, lambda e: e.activation(out=hct[:, n0:n0 + n], in_=psb[pb][:, 0:n], func=AF.Copy),
                                 reads=[pbuf[pb]], writes=[bhct])
                        fm_block(3072 + j * 128, ntiles, ev_hc)
                        K.op(DVE, lambda e: e.tensor_copy(out=uf[:, 0:2], in_=uprev[:, j, :]), reads=[buprev],
                             writes=[buf_])
                        for s_ in range(4):
                            K.dma(POOL, uf[:, 2050 + 18 * s_:2052 + 18 * s_],
                                  state_conv[s_, :, j * 128:(j + 1) * 128].rearrange("r p -> p r"), writes=[buf_])

                        def ev_gc(pb, n0, n):
                            if n0 < 2048:
                                K.op(DVE, lambda e: e.tensor_tensor(out=uf[:, 2 + n0:2 + n0 + n], in0=psb[pb][:, 0:n],
                                                                    in1=hct[:, n0:n0 + n], op=ALU.mult),
                                     reads=[pbuf[pb], bhct], writes=[buf_])
                            else:
                                for s_ in range(4):
                                    K.op(DVE, lambda e, s_=s_: e.tensor_tensor(
                                        out=uf[:, 2052 + 18 * s_:2068 + 18 * s_], in0=psb[pb][:, 16 * s_:16 * s_ + 16],
                                        in1=hct[:, 2048 + 16 * s_:2064 + 16 * s_], op=ALU.mult),
                                         reads=[pbuf[pb], bhct], writes=[buf_])
                        fm_block(5120 + j * 128, ntiles, ev_gc)
                        for si, (uoff, coff, L) in enumerate(segs):
                            K.op(DVE, lambda e: e.tensor_scalar(out=cv[:, coff:coff + L], in0=uf[:, uoff:uoff + L],
                                                                 scalar1=cw[:, 0, j:j + 1], scalar2=None, op0=ALU.mult),
                                 reads=[buf_, cst], writes=[bcv])
                            for r in (1, 2):
                                K.op(DVE, lambda e, r=r: e.scalar_tensor_tensor(
                                    out=cv[:, coff:coff + L], in0=uf[:, uoff + r:uoff + r + L], scalar=cw[:, r, j:j + 1],
                                    in1=cv[:, coff:coff + L], op0=ALU.mult, op1=ALU.add), reads=[buf_, cst, bcv],
                                     writes=[bcv])
                            K.dma(POOL, nconv_out[si, :, j * 128:(j + 1) * 128].rearrange("r p -> p r"),
                                  uf[:, uoff + L:uoff + L + 2], reads=[buf_], is_output=True)

                        def ev_gb(pb, n0, n, ob, bob):
                            K.op(DVE, lambda e: e.tensor_tensor(out=ob[:, n0:n0 + n], in0=psb[pb][:, 0:n],
                                                                in1=cv[:, n0:n0 + n], op=ALU.mult),
                                 reads=[pbuf[pb], bcv], writes=[bob])
                        simple_block(4096 + j * 128, ev_gb, GC_d[j, :, :], dbufs["GC"])
                    if CUT <= 3:
                        K.barrier()
                        return
                    for j in range(16):
                        simple_block(6144 + j * 128, ev_sigmoid, SG_d[0, j, :, :], dbufs["SG"])
                    for j in range(16):
                        simple_block(8192 + j * 128, ev_sigmoid, SG_d[1, j, :, :], dbufs["SG"])
                K.barrier()
            if CUT <= 4 or (CUT == 5 and not is_ctx) or (CUT == 6 and is_ctx):
                return
            with ExitStack() as st3:
                def sb3(name, shape, dt=F32):
                    return st3.enter_context(nc.sbuf_tensor(name + ("c" if is_ctx else "o"), list(shape), dt))
                wst2 = [sb3(f"wst2{i}", [128, 16, 512]) for i in range(1)] * 2
                wbf2 = [sb3(f"wbf2{i}", [128, 16, 512], BF16) for i in range(2)]
                bwst2 = [Buf()] * 2
                bwbf2 = [Buf(), Buf()]
                stf = [sb3(f"stf{i}", [128, 512]) for i in range(4)]
                bstf = [Buf() for _ in range(4)]
                stb = [sb3(f"stb{i}", [128, 512], BF16) for i in range(4)]
                bstb = [Buf() for _ in range(4)]
                gi = 0
                ei = 0
                groups = ([] if is_ctx else [("k", g) for g in range(2)]) + [("v", g) for g in range(2)]
                for (kind, g) in groups:
                    col0 = (1024 if kind == "k" else 2048) + g * 512
                    ws, wb, bws, bwb = wst2[gi % 2], wbf2[gi % 2], bwst2[gi % 2], bwbf2[gi % 2]
                    gi += 1
                    K.dma(SP, ws[:], w_in[:, col0:col0 + 512].rearrange("(c p) n -> p c n", p=128), writes=[bws])
                    K.op(DVE, lambda e: e.tensor_copy(out=wb[:], in_=ws[:]), reads=[bws], writes=[bwb])
                    for (t0, tp) in tblocks:
                        pb = nextbank()
                        for c in range(16):
                            K.op(PE, lambda e, c=c: e.matmul(psb[pb][0:tp, 0:512], lhsT=hT_sb[:, c, t0:t0 + tp],
                                                             rhs=wb[:, c, :], start=(c == 0), stop=(c == 15)),
                                 reads=[bwb, bhT[c]], writes=[pbuf[pb]], signal=(c == 15))
                        sf, bsf, sbb, bsb = stf[ei % 4], bstf[ei % 4], stb[ei % 4], bstb[ei % 4]
                        ei += 1
                        if not is_ctx:
                            K.op(DVE, lambda e: e.tensor_copy(out=sf[0:tp, :], in_=psb[pb][0:tp, 0:512]),
                                 reads=[pbuf[pb]], writes=[bsf])
                            dst = nk_out if kind == "k" else nv_out
                            K.dma(POOL, dst[t0:t0 + tp, g * 512:(g + 1) * 512], sf[0:tp, :], reads=[bsf], is_output=True)
                        if kind == "v":
                            K.op(ACT, lambda e: e.activation(out=sbb[0:tp, :], in_=psb[pb][0:tp, 0:512], func=AF.Copy),
                                 reads=[pbuf[pb]], writes=[bsb])
                            K.dma(POOL, V_d[tok0 + t0:tok0 + t0 + tp, g * 512:(g + 1) * 512], sbb[0:tp, :], reads=[bsb],
                                  writes=[dbufs["V"]])
                K.barrier()

    proj_pass(True)
    proj_pass(False)
    if stop_after <= 2:
        K.finish()
        return nc


    def run_sb_pipeline(st, tag, tiles, classes):
        def sb(name, shape, dt=F32):
            return st.enter_context(nc.sbuf_tensor(name + tag, list(shape), dt))
        NE = 4
        pools = {}
        for cls, (nq_, banks_) in classes.items():
            pools[cls] = dict(
                nq=nq_, banks=banks_, ctr=0,
                ebuf=[sb(f"ebuf{cls}{i}", [128, nq_]) for i in range(NE)], bebuf=[Buf() for _ in range(NE)],
                Lb=[sb(f"Lb{cls}{i}", [128, nq_], BF16) for i in range(NE)], bLb=[Buf() for _ in range(NE)],
                ab=[sb(f"ab{cls}{i}", [128, nq_], BF16) for i in range(NE)], bab=[Buf() for _ in range(NE)])
        n = len(tiles)
        for t in tiles:
            P_ = pools[t["cls"]]
            t["k"] = P_["ctr"]
            P_["ctr"] += 1

        def stS(i):
            t = tiles[i]
            P_ = pools[t["cls"]]
            t["bk"] = P_["banks"][t["k"] % len(P_["banks"])]
            t["s_mm"](t["bk"])

        def stA(i):
            t = tiles[i]
            P_ = pools[t["cls"]]
            nq, k = P_["nq"], t["k"]
            bk, nk = t["bk"], t["nkeys"]
            eb, beb, lb, blb = P_["ebuf"][k % NE], P_["bebuf"][k % NE], P_["Lb"][k % NE], P_["bLb"][k % NE]
            bias_ap = t["bias"]
            K.op(ACT, lambda e: e.activation(out=eb[0:nk, :], in_=psb[bk][0:nk, 0:nq], func=AF.Exp,
                                             bias=bias_ap[0:nk, :]), reads=[pbuf[bk], cst], writes=[beb])
            K.op(ACT, lambda e: e.activation(out=lb[0:nk, :], in_=eb[0:nk, :], func=AF.Ln, bias=1.0),
                 reads=[beb], writes=[blb])
            if t["mask"] is not None:
                K.op(POOL, lambda e: e.tensor_tensor(out=lb[0:nk, :], in0=lb[0:nk, :], in1=t["mask"], op=ALU.mult),
                     reads=[blb, cst], writes=[blb])
            Lacc, bLacc, Laccb, bLaccb = t["lacc"]
            first = t["first"]
            rd = [blb, cst]
            if not first:
                ci = t["ci"]
                lab, blab = Laccb[ci % 2], bLaccb[ci % 2]
                K.op(DVE, lambda e: e.tensor_copy(out=lab[:, :], in_=Lacc[:, :]), reads=[bLacc], writes=[blab])
                rd.append(blab)
            K.op(PE, lambda e: e.matmul(psb[bk][0:nk, 0:nq], lhsT=nti[0:nk, 0:nk], rhs=lb[0:nk, :],
                                        start=False, stop=first, skip_group_check=True),
                 reads=rd, writes=[pbuf[bk]], signal=first)
            if not first:
                K.op(PE, lambda e: e.matmul(psb[bk][0:nk, 0:nq], lhsT=neg1[:, 0:nk], rhs=lab[:, :],
                                            start=False, stop=True, skip_group_check=True),
                     reads=rd, writes=[pbuf[bk]])
            if first:
                if nk < 128:
                    K.op(DVE, lambda e: e.memset(Lacc[:, :], 0.0), writes=[bLacc])
                K.op(DVE, lambda e: e.tensor_copy(out=Lacc[0:nk, :], in_=lb[0:nk, :]), reads=[blb], writes=[bLacc])
            else:
                K.op(DVE, lambda e: e.tensor_tensor(out=Lacc[0:nk, :], in0=Lacc[0:nk, :], in1=lb[0:nk, :], op=ALU.add),
                     reads=[blb, bLacc], writes=[bLacc])

        def stB(i):
            t = tiles[i]
            P_ = pools[t["cls"]]
            nq, k = P_["nq"], t["k"]
            bk, nk = t["bk"], t["nkeys"]
            aa, baa = P_["ab"][k % NE], P_["bab"][k % NE]
            bias_ap = t["bias"]
            K.op(ACT, lambda e: e.activation(out=aa[0:nk, :], in_=psb[bk][0:nk, 0:nq], func=AF.Exp,
                                             bias=bias_ap[0:nk, :]), reads=[pbuf[bk], cst], writes=[baa])
            if t["mask"] is not None:
                K.op(POOL, lambda e: e.tensor_tensor(out=aa[0:nk, :], in0=aa[0:nk, :], in1=t["mask"], op=ALU.mult),
                     reads=[baa, cst], writes=[baa])
            t["av_mm"](aa, baa)
            if t.get("on_last") is not None:
                t["on_last"]()

        LA = 12
        for s_ in range(-LA, n):
            if 0 <= s_ + LA < n and tiles[s_ + LA].get("prep") is not None:
                tiles[s_ + LA]["prep"]()
            if 0 <= s_ + 2 < n:
                stS(s_ + 2)
            if 0 <= s_ + 1 < n:
                stA(s_ + 1)
            if 0 <= s_ < n:
                stB(s_)

    with ExitStack() as st:
        def sb(name, shape, dt=F32):
            return st.enter_context(nc.sbuf_tensor(name, list(shape), dt))
        KTh = [sb(f"KTh{i}", [128, 4096], BF16) for i in range(2)]
        Vh = [sb(f"Vh{i}", [128, 32, 128], BF16) for i in range(2)]
        QTh = [sb(f"QTh{i}", [128, 2048], BF16) for i in range(2)]
        bKV = [Buf(), Buf()]
        LaccP = [sb(f"LaccP{i}", [128, 512]) for i in range(2)]
        bLaccP = [Buf(), Buf()]
        LaccbP = [[sb(f"LaccbP{i}{j}", [128, 512], BF16) for j in range(2)] for i in range(2)]
        bLaccbP = [[Buf(), Buf()], [Buf(), Buf()]]
        ot = [sb(f"ot{i}", [128, 512], BF16) for i in range(2)]
        bot = [Buf(), Buf()]
        tiles = []
        qctr = 0
        for h in range(8):
            kt, vh, qt, bkv = KTh[h % 2], Vh[h % 2], QTh[h % 2], bKV[h % 2]

            def prep_head(h=h, kt=kt, vh=vh, qt=qt, bkv=bkv):
                K.dma(SP, kt[:], KT_d[h, :, 0:4096], reads=[dbufs["KT"]], writes=[bkv])
                K.dma(SP, vh[:], V_d[0:4096, h * 128:(h + 1) * 128].rearrange("(b p) d -> p b d", p=128),
                      reads=[dbufs["V"]], writes=[bkv])
                K.dma(SP, qt[:], QT_d[h, :, 0:2048], reads=[dbufs["QT"]], writes=[bkv])
            for qti in range(4):
                ob = 3 + (qctr % 2)
                li = qctr % 2
                qctr += 1
                q0 = qti * 512
                nkb = 16 + 4 * qti + 4
                for r, kb in enumerate(range(nkb - 1, -1, -1)):
                    first = (r == 0)
                    last = (kb == 0)
                    m = kb - (16 + 4 * qti)

                    def s_mm(bk, kb=kb, kt=kt, qt=qt, bkv=bkv, q0=q0):
                        K.op(PE, lambda e: e.matmul(psb[bk][:, :], lhsT=kt[:, kb * 128:(kb + 1) * 128],
                                                    rhs=qt[:, q0:q0 + 512], start=True, stop=False,
                                                    skip_group_check=True), reads=[bkv], writes=[pbuf[bk]])

                    def av_mm(aa, baa, kb=kb, first=first, last=last, vh=vh, bkv=bkv, ob=ob):
                        K.op(PE, lambda e: e.matmul(psb[ob][:, :], lhsT=vh[:, kb, :], rhs=aa[:, :], start=first,
                                                    stop=last, skip_group_check=True), reads=[bkv, baa],
                             writes=[pbuf[ob]])

                    def on_last(h=h, q0=q0, ob=ob, qti=qti):
                        o_t, bo_t = ot[qti % 2], bot[qti % 2]
                        K.op(DVE, lambda e: e.tensor_copy(out=o_t[:], in_=psb[ob][:, :]), reads=[pbuf[ob]],
                             writes=[bo_t])
                        K.dma(POOL, OT_d[h, :, q0:q0 + 512], o_t[:], reads=[bo_t], writes=[dbufs["OT"]])
                    tiles.append(dict(
                        cls="p", nkeys=128, first=first, ci=r, mask=(maskd[:, m, :] if m >= 0 else None),
                        bias=(flg[:, 0:1] if kb < 16 else zero1), s_mm=s_mm, av_mm=av_mm,
                        lacc=(LaccP[li], bLaccP[li], LaccbP[li], bLaccbP[li]),
                        on_last=(on_last if last else None),
                        prep=(prep_head if (qti == 0 and first) else None)))
        ptiles = tiles
        LaccS = [sb(f"LaccS{i}", [128, 128]) for i in range(4)]
        bLaccS = [Buf() for _ in range(4)]
        LaccbS = [[sb(f"LaccbS{i}{j}", [128, 128], BF16) for j in range(2)] for i in range(4)]
        bLaccbS = [[Buf(), Buf()] for _ in range(4)]
        NKF = 4
        kf = [sb(f"kf{i}", [128, 1024]) for i in range(NKF)]
        vf = [sb(f"vf{i}", [128, 1024]) for i in range(NKF)]
        bkf = [Buf() for _ in range(NKF)]
        bvf = [Buf() for _ in range(NKF)]
        kbf = [sb(f"kbf{i}", [128, 1024], BF16) for i in range(3)]
        bkbf = [Buf() for _ in range(3)]
        NV = 8
        vbf = [sb(f"vbf{i}", [128, 1024], BF16) for i in range(NV)]
        bvbf = [Buf() for _ in range(NV)]
        ktb = [sb(f"ktb{i}", [128, 8, 128], BF16) for i in range(NV)]
        bktb = [Buf() for _ in range(NV)]
        qs = sb("qs", [128, 8, 64], BF16)
        bqs = Buf()
        ktn = sb("ktn", [128, 8, 64], BF16)
        vn = sb("vn", [16, 4, 1024], BF16)
        bnew = Buf()
        ots = sb("ots", [128, 4, 128], BF16)
        bots = Buf()
        for h in range(8):
            K.dma(SP, qs[:, h, :], QT_d[h, :, 2048:2112], reads=[dbufs["QT"]], writes=[bqs])
            K.dma(SP, ktn[:, h, :], KT_d[h, :, 4096:4160], reads=[dbufs["KT"]], writes=[bnew])
        for sq in range(4):
            K.dma(SP, vn[:, sq, :], V_d[4096 + 16 * sq:4112 + 16 * sq, :], reads=[dbufs["V"]], writes=[bnew])
        OB = 7
        tiles = []
        li = 0
        for r in range(33):
            for sq in range(4):
                first = (r == 0)
                last = (r == 32)
                if first:
                    nkeys = 16
                    mask_ap = masks[:].rearrange("p h i -> p (h i)")
                    prep = None
                    kti = bkti = vbi = bvbi = None
                else:
                    kb = 32 - r
                    nkeys = 128
                    mask_ap = None
                    kfi, bkfi, vfi, bvfi = kf[li % NKF], bkf[li % NKF], vf[li % NKF], bvf[li % NKF]
                    kbi, bkbi = kbf[li % 3], bkbf[li % 3]
                    vbi, bvbi = vbf[li % NV], bvbf[li % NV]
                    kti, bkti = ktb[li % NV], bktb[li % NV]
                    tbk = 5
                    li += 1

                    def prep(sq=sq, kb=kb, kfi=kfi, bkfi=bkfi, vfi=vfi, bvfi=bvfi, kbi=kbi, bkbi=bkbi, vbi=vbi,
                             bvbi=bvbi, kti=kti, bkti=bkti, tbk=tbk):
                        K.dma(SP, kfi[:], cache_k[sq, kb * 128:(kb + 1) * 128, :], writes=[bkfi])
                        K.dma(POOL, vfi[:], cache_v[sq, kb * 128:(kb + 1) * 128, :], writes=[bvfi])
                        K.op(DVE, lambda e: e.tensor_copy(out=kbi[:], in_=kfi[:]), reads=[bkfi], writes=[bkbi])
                        K.op(ACT, lambda e: e.activation(out=vbi[:], in_=vfi[:], func=AF.Copy), reads=[bvfi],
                             writes=[bvbi])
                        pst = psb[tbk][:].bitcast(BF16)
                        for h in range(8):
                            K.op(PE, lambda e, h=h: e.transpose(out=pst[:, h * 128:(h + 1) * 128],
                                                                in_=kbi[:, h * 128:(h + 1) * 128], identity=ident[:]),
                                 reads=[bkbi, cst], writes=[pbuf[tbk]], signal=(h == 7))
                        K.op(DVE, lambda e: e.tensor_copy(out=kti[:].rearrange("p h k -> p (h k)"), in_=pst[:, :]),
                             reads=[pbuf[tbk]], writes=[bkti])

                def s_mm(bk, first=first, sq=sq, nkeys=nkeys, kti=kti, bkti=bkti):
                    for h in range(8):
                        if first:
                            lhsT = ktn[:, h, 16 * sq:16 * sq + 16]
                            rds = [bnew, bqs]
                        else:
                            lhsT = kti[:, h, :]
                            rds = [bkti, bqs]
                        K.op(PE, lambda e, h=h, lhsT=lhsT: e.matmul(psb[bk][0:nkeys, h * 16:(h + 1) * 16], lhsT=lhsT,
                                                                    rhs=qs[:, h, 16 * sq:16 * sq + 16], start=(h == 0),
                                                                    stop=False, skip_group_check=True),
                             reads=rds, writes=[pbuf[bk]], signal=(h == 7))

                def av_mm(aa, baa, first=first, last=last, sq=sq, nkeys=nkeys, vbi=vbi, bvbi=bvbi, r=r):
                    for h in range(8):
                        if first:
                            lhsT = vn[0:16, sq, h * 128:(h + 1) * 128]
                            rds = [bnew, baa]
                        else:
                            lhsT = vbi[:, h * 128:(h + 1) * 128]
                            rds = [bvbi, baa]
                        c0 = sq * 128 + h * 16
                        K.op(PE, lambda e, h=h, lhsT=lhsT, c0=c0: e.matmul(
                            psb[OB][:, c0:c0 + 16], lhsT=lhsT, rhs=aa[0:nkeys, h * 16:(h + 1) * 16],
                            start=(first and sq == 0 and h == 0), stop=last, skip_group_check=True),
                             reads=rds, writes=[pbuf[OB]], signal=(h == 7))

                def on_last_all():
                    K.op(DVE, lambda e: e.tensor_copy(out=ots[:].rearrange("p s c -> p (s c)"), in_=psb[OB][:, :]),
                         reads=[pbuf[OB]], writes=[bots])
                    for h in range(8):
                        for sq_ in range(4):
                            K.dma(POOL, OT_d[h, :, 2048 + 16 * sq_:2064 + 16 * sq_], ots[:, sq_, h * 16:(h + 1) * 16],
                                  reads=[bots], writes=[dbufs["OT"]])
                tiles.append(dict(
                    cls="s", nkeys=nkeys, first=first, ci=r, mask=mask_ap, bias=zero1, s_mm=s_mm, av_mm=av_mm,
                    lacc=(LaccS[sq], bLaccS[sq], LaccbS[sq], bLaccbS[sq]),
                    on_last=(on_last_all if (last and sq == 3) else None), prep=prep))
        stiles = tiles
        merged = []
        si_ = 0
        for pi_, t_ in enumerate(ptiles):
            merged.append(t_)
            if pi_ % 6 == 5 and si_ < len(stiles):
                merged.append(stiles[si_])
                si_ += 1
        merged += stiles[si_:]
        run_sb_pipeline(st, "m", merged, {"p": (512, (0, 1, 2, 6)), "s": (128, (5,))})
        K.barrier()
    if stop_after <= 3:
        K.finish()
        return nc

    own_tiles = [(i * 512, 512) for i in range(4)] + [(2048, 64)]
    own_blocks = [(i * 128, 128) for i in range(16)] + [(2048, 64)]
    with ExitStack() as st:
        def sb(name, shape, dt=F32):
            return st.enter_context(nc.sbuf_tensor(name, list(shape), dt))
        OT_sb = sb("OT_sb", [128, 8, NOWN], BF16)
        GC_sb = sb("GC_sb", [128, 8, NOWN], BF16)
        bOT = [Buf() for _ in range(8)]
        bGC = [Buf() for _ in range(8)]
        for c in range(8):
            K.dma(SP, OT_sb[:, c, :], OT_d[c, :, :], reads=[dbufs["OT"]], writes=[bOT[c]])
            K.dma(POOL, GC_sb[:, c, :], GC_d[c, :, :], reads=[dbufs["GC"]], writes=[bGC[c]])
        wf = [sb(f"wf{i}", [128, 2, 8, 128]) for i in range(2)]
        wb_ = [sb(f"wb{i}", [128, 2, 8, 128], BF16) for i in range(2)]
        bwf = [Buf(), Buf()]
        bwb = [Buf(), Buf()]
        sg = [sb(f"sg{i}", [128, 2, NOWN], BF16) for i in range(2)]
        bsg = [Buf(), Buf()]
        mgt = [sb(f"mgt{i}", [128, NOWN], BF16) for i in range(2)]
        bmgt = [Buf(), Buf()]
        t1 = [sb(f"t1{i}", [128, 512]) for i in range(2)]
        t2 = [sb(f"t2{i}", [128, 512]) for i in range(2)]
        bt1 = [Buf(), Buf()]
        bt2 = [Buf(), Buf()]
        ti = 0
        for j in range(16):
            f_, b_, bf_, bb_ = wf[j % 2], wb_[j % 2], bwf[j % 2], bwb[j % 2]
            K.dma(SP, f_[:, 0, :, :], w_ap[:, j * 128:(j + 1) * 128].rearrange("(c p) n -> p c n", p=128), writes=[bf_])
            K.dma(SP, f_[:, 1, :, :], w_co[:, j * 128:(j + 1) * 128].rearrange("(c p) n -> p c n", p=128), writes=[bf_])
            K.op(ACT, lambda e: e.activation(out=b_[:], in_=f_[:], func=AF.Copy), reads=[bf_], writes=[bb_])
            sg_, bsg_ = sg[j % 2], bsg[j % 2]
            K.dma(SP, sg_[:, 0, :], SG_d[0, j, :, :], reads=[dbufs["SG"]], writes=[bsg_])
            K.dma(SP, sg_[:, 1, :], SG_d[1, j, :, :], reads=[dbufs["SG"]], writes=[bsg_])
            mg_, bmg_ = mgt[j % 2], bmgt[j % 2]
            for (n0, n) in own_tiles:
                pa, pc = nextbank(), nextbank()
                for c in range(8):
                    K.op(PE, lambda e, c=c: e.matmul(psb[pa][:, 0:n], lhsT=b_[:, 0, c, :], rhs=OT_sb[:, c, n0:n0 + n],
                                                     start=(c == 0), stop=(c == 7)), reads=[bb_, bOT[c]],
                         writes=[pbuf[pa]], signal=(c == 7))
                for c in range(8):
                    K.op(PE, lambda e, c=c: e.matmul(psb[pc][:, 0:n], lhsT=b_[:, 1, c, :], rhs=GC_sb[:, c, n0:n0 + n],
                                                     start=(c == 0), stop=(c == 7)), reads=[bb_, bGC[c]],
                         writes=[pbuf[pc]], signal=(c == 7))
                a1, a2, ba1, ba2 = t1[ti % 2], t2[ti % 2], bt1[ti % 2], bt2[ti % 2]
                ti += 1
                K.op(DVE, lambda e: e.tensor_tensor(out=a1[:, 0:n], in0=psb[pa][:, 0:n], in1=sg_[:, 0, n0:n0 + n],
                                                    op=ALU.mult), reads=[pbuf[pa], bsg_], writes=[ba1])
                K.op(DVE, lambda e: e.tensor_tensor(out=a2[:, 0:n], in0=psb[pc][:, 0:n], in1=sg_[:, 1, n0:n0 + n],
                                                    op=ALU.mult), reads=[pbuf[pc], bsg_], writes=[ba2])
                K.op(POOL, lambda e: e.tensor_tensor(out=mg_[:, n0:n0 + n], in0=a1[:, 0:n], in1=a2[:, 0:n], op=ALU.add),
                     reads=[ba1, ba2], writes=[bmg_])
            K.dma(POOL, MG_d[j, :, :], mg_[:], reads=[bmg_], writes=[dbufs["MG"]])
        K.barrier()
    with ExitStack() as st:
        def sb(name, shape, dt=F32):
            return st.enter_context(nc.sbuf_tensor(name, list(shape), dt))
        wob = sb("wob", [128, 16, D], BF16)
        bwob = [Buf() for _ in range(16)]
        stg = [sb(f"stg{i}", [128, D]) for i in range(2)]
        bstg = [Buf(), Buf()]
        for c in range(16):
            K.dma(SP, stg[c % 2][:], w_o[c * 128:(c + 1) * 128, :], writes=[bstg[c % 2]])
            K.op(DVE if c % 2 == 0 else ACT,
                 (lambda e, c=c: e.tensor_copy(out=wob[:, c, :], in_=stg[c % 2][:])) if c % 2 == 0 else
                 (lambda e, c=c: e.activation(out=wob[:, c, :], in_=stg[c % 2][:], func=AF.Copy)),
                 reads=[bstg[c % 2]], writes=[bwob[c]])
        G1 = sb("G1", [128, D])
        bG1 = Buf()
        K.dma(SP, G1[:], gS_d[0, 0, :].unsqueeze(0).to_broadcast([128, D]), reads=[dbufs["gS"]], writes=[bG1])
        xts = [sb(f"x4t{i}", [128, D]) for i in range(2)]
        bxts = [Buf(), Buf()]
        tq = [sb(f"tq{i}", [128, 512]) for i in range(2)]
        btq = [Buf(), Buf()]
        mgl = [sb(f"mgl{i}", [128, 16, 128], BF16) for i in range(2)]
        bmgl = [Buf(), Buf()]
        h2t = [sb(f"h2t{i}", [128, 16, 128], BF16) for i in range(2)]
        bh2t = [Buf(), Buf()]
        nctx2 = NormCtx(st, "b")
        qi = 0
        def p4_front(tb):
            nonlocal_qi = qi_box
            t0, tp = own_blocks[tb]
            if tb == 16:
                for s_ in range(4):
                    K.dma(SP, G1[16 * s_:16 * s_ + 16, :], gS_d[0, 1 + s_, :].unsqueeze(0).to_broadcast([16, D]),
                          reads=[dbufs["gS"]], writes=[bG1])
            ml, bml = mgl[tb % 2], bmgl[tb % 2]
            K.dma(SP, ml[:, :, 0:tp], MG_d[:, :, t0:t0 + tp].rearrange("c p t -> p c t"), reads=[dbufs["MG"]],
                  writes=[bml])
            xt, bxt = xts[tb % 2], bxts[tb % 2]
            K.dma(SP, xt[0:tp, :], xall[NCTX + t0:NCTX + t0 + tp, :], writes=[bxt])
            for nq in range(4):
                pb = nq
                for c in range(16):
                    K.op(PE, lambda e, c=c: e.matmul(psb[pb][0:tp, :], lhsT=ml[:, c, 0:tp],
                                                     rhs=wob[:, c, nq * 512:(nq + 1) * 512], start=(c == 0),
                                                     stop=(c == 15)), reads=[bml, bwob[c]], writes=[pbuf[pb]],
                         signal=(c == 15))
                tt, btt = tq[nonlocal_qi[0] % 2], btq[nonlocal_qi[0] % 2]
                nonlocal_qi[0] += 1
                K.op(DVE, lambda e: e.tensor_tensor(out=tt[0:tp, :], in0=psb[pb][0:tp, :],
                                                    in1=G1[0:tp, nq * 512:(nq + 1) * 512], op=ALU.mult),
                     reads=[pbuf[pb], bG1], writes=[btt])
                K.op(POOL, lambda e: e.tensor_tensor(out=xt[0:tp, nq * 512:(nq + 1) * 512],
                                                     in0=xt[0:tp, nq * 512:(nq + 1) * 512], in1=tt[0:tp, :], op=ALU.add),
                     reads=[btt, bxt], writes=[bxt])
            K.dma(POOL, X1_d[t0:t0 + tp, :], xt[0:tp, :], reads=[bxt], writes=[dbufs["X1"]])
            return emit_norm_stats(nctx2, xt, bxt, tp)

        def p4_back(tb, st_):
            t0, tp = own_blocks[tb]
            ht, bht = h2t[tb % 2], bh2t[tb % 2]
            emit_norm_tr(nctx2, st_, tp, seq_groups(16 + tb), A2, B2, ht, bht, 0, (4 + 2 * (tb % 2), 5 + 2 * (tb % 2)))
            K.dma(POOL, H2_d[:, :, t0:t0 + tp].rearrange("c p t -> p c t"), ht[:, :, 0:tp], reads=[bht],
                  writes=[dbufs["H2"]])

        qi_box = [0]
        pend = p4_front(0)
        for tb in range(len(own_blocks)):
            cur = pend
            if tb + 1 < len(own_blocks):
                pend = p4_front(tb + 1)
            p4_back(tb, cur)
        K.barrier()
    if stop_after <= 4:
        K.finish()
        return nc


    with ExitStack() as st:
        def sb(name, shape, dt=F32):
            return st.enter_context(nc.sbuf_tensor(name, list(shape), dt))
        h2T = sb("h2T", [128, 16, NOWN], BF16)
        bh2T = [Buf() for _ in range(16)]
        for c in range(16):
            K.dma(SP if c % 2 == 0 else POOL, h2T[:, c, :], H2_d[c, :, :], reads=[dbufs["H2"]], writes=[bh2T[c]])
        identf = sb("identf", [128, 128])
        bidf = Buf()
        K.op(POOL, lambda e: e.memset(identf[:], 0.0), writes=[bidf])
        K.op(POOL, lambda e: e.affine_select(out=identf[:], in_=identf[:], pattern=[[-1, 128]],
                                             compare_op=ALU.not_equal, fill=1.0, base=0, channel_multiplier=1),
             reads=[bidf], writes=[bidf])
        sk_sb = sb("sk_sb", [128, 16, 128])
        bsk = Buf()
        K.dma(SP, sk_sb[:], sub_keys.rearrange("g k d -> k g d"), writes=[bsk])
        SKT = sb("SKT", [128, 16, 128])
        bSKT = Buf()
        for g4 in range(4):
            pb = nextbank()
            for gg in range(4):
                g = g4 * 4 + gg
                K.op(PE, lambda e, g=g, gg=gg: e.matmul(psb[pb][:, gg * 128:(gg + 1) * 128], lhsT=sk_sb[:, g, :],
                                                        rhs=identf[:], start=True, stop=True),
                     reads=[bsk, bidf], writes=[pbuf[pb]], signal=(gg == 3))
            K.op(DVE, lambda e: e.tensor_copy(out=SKT[:, g4 * 4:(g4 + 1) * 4, :].rearrange("p g k -> p (g k)"),
                                              in_=psb[pb][:, :]), reads=[pbuf[pb]], writes=[bSKT])
        wqf = [sb(f"wqf{i}", [128, 16, 128]) for i in range(2)]
        wqb = [sb(f"wqb{i}", [128, 16, 128], BF16) for i in range(2)]
        bwqf = [Buf(), Buf()]
        bwqb = [Buf(), Buf()]
        qTg = [sb(f"qTg{i}", [128, NOWN]) for i in range(2)]
        bqTg = [Buf(), Buf()]
        sst = [sb(f"sst{i}", [128, 17, 128]) for i in range(2)]
        bsst = [Buf(), Buf()]
        for g in range(16):
            f_, b_, bf_, bb_ = wqf[g % 2], wqb[g % 2], bwqf[g % 2], bwqb[g % 2]
            K.dma(SP, f_[:], w_q[:, g * 128:(g + 1) * 128].rearrange("(c p) n -> p c n", p=128), writes=[bf_])
            K.op(DVE, lambda e: e.tensor_copy(out=b_[:], in_=f_[:]), reads=[bf_], writes=[bb_])
            qg, bqg = qTg[g % 2], bqTg[g % 2]
            for (n0, n) in own_tiles:
                pb = nextbank()
                for c in range(16):
                    K.op(PE, lambda e, c=c: e.matmul(psb[pb][:, 0:n], lhsT=b_[:, c, :], rhs=h2T[:, c, n0:n0 + n],
                                                     start=(c == 0), stop=(c == 15)), reads=[bb_, bh2T[c]],
                         writes=[pbuf[pb]], signal=(c == 15))
                K.op(ACT, lambda e: e.activation(out=qg[:, n0:n0 + n], in_=psb[pb][:, 0:n], func=AF.Copy),
                     reads=[pbuf[pb]], writes=[bqg])
            ss_, bss_ = sst[g % 2], bsst[g % 2]
            for b4 in range(5):
                pb = nextbank()
                blks = own_blocks[b4 * 4:(b4 + 1) * 4]
                for bi, (t0, tp) in enumerate(blks):
                    K.op(PE, lambda e, bi=bi, t0=t0, tp=tp: e.matmul(psb[pb][0:tp, bi * 128:(bi + 1) * 128],
                                                                     lhsT=qg[:, t0:t0 + tp], rhs=SKT[:, g, :],
                                                                     start=True, stop=True),
                         reads=[bqg, bSKT], writes=[pbuf[pb]], signal=(bi == len(blks) - 1))
                tpm = blks[0][1]
                nb = len(blks)
                K.op(DVE, lambda e: e.tensor_copy(out=ss_[0:tpm, b4 * 4:b4 * 4 + nb, :],
                                                  in_=psb[pb][0:tpm, 0:nb * 128].rearrange("p (b k) -> p b k", b=nb)),
                     reads=[pbuf[pb]], writes=[bss_])
            K.dma(POOL, S_d[0:16, :, g * 128:(g + 1) * 128].rearrange("b p k -> p b k"), ss_[:, 0:16, :], reads=[bss_],
                  writes=[dbufs["S"]])
            K.dma(POOL, S_d[16, 0:64, g * 128:(g + 1) * 128], ss_[0:64, 16, :], reads=[bss_], writes=[dbufs["S"]])
        K.barrier()
    if stop_after <= 5:
        K.finish()
        return nc

    with ExitStack() as st:
        def sb(name, shape, dt=F32):
            return st.enter_context(nc.sbuf_tensor(name, list(shape), dt))
        s_sbs = [sb(f"s_sb{i}", [128, 16, 128]) for i in range(2)]
        bss_ = [Buf(), Buf()]
        v16 = sb("v16", [128, 16, 16])
        bv = Buf()
        tmpA2 = [sb(f"tmpA{i}", [128, 128]) for i in range(2)]
        btA2 = [Buf(), Buf()]
        joinj = sb("joinj", [128, 1])
        bvg = [Buf() for _ in range(16)]
        bcth = [Buf() for _ in range(8)]
        cand = sb("cand", [128, 8, 256])
        bcand = Buf()
        tmpB2 = [sb(f"tmpB{i}", [128, 256]) for i in range(2)]
        btB2 = [Buf(), Buf()]
        ctop = sb("ctop", [128, 8, 16])
        cidx = sb("cidx", [128, 8, 16], U32)
        bct = Buf()
        gate = sb("gate", [128, 8, 16])
        zs = sb("zs", [128, 8])
        bgate = Buf()
        iu = sb("iu", [128, 8, 16], U32)
        ju = sb("ju", [128, 8, 16], U32)
        i_f = sb("i_f", [128, 8, 16])
        j_f = sb("j_f", [128, 8, 16])
        bij = Buf()
        oh = sb("oh", [128, 8, 16, 16])
        boh = Buf()
        vsel = sb("vsel", [128, 2, 8, 16])
        bvsel = Buf()
        E1s = [sb(f"E1_{i}", [128, 8, 16, 32], BF16) for i in range(2)]
        E2s = [sb(f"E2_{i}", [128, 8, 16, 32], BF16) for i in range(2)]
        bE1s = [Buf(), Buf()]
        bE2s = [Buf(), Buf()]
        XT1 = sb("XT1", [128, 128, 128], BF16)
        XT2 = sb("XT2", [128, 128, 128], BF16)
        bXT1 = Buf()
        bXT2 = Buf()
        Gsb = sb("Gsb", [128, 128, 128], BF16)
        bGsb = Buf()
        K.op(POOL, lambda e: e.memset(Gsb[:], 0.0), writes=[bGsb])
        ectr = 0
        for tb, (t0, tp) in enumerate(own_blocks):
            s_sb, bs = s_sbs[tb % 2], bss_[tb % 2]
            if tb == 0:
                K.dma(SP, s_sb[0:tp, :, :].rearrange("p g k -> p (g k)"), S_d[0, 0:tp, :], reads=[dbufs["S"]], writes=[bs])
            if tb + 1 < len(own_blocks):
                tpn = own_blocks[tb + 1][1]
                K.dma(SP, s_sbs[(tb + 1) % 2][0:tpn, :, :].rearrange("p g k -> p (g k)"), S_d[tb + 1, 0:tpn, :],
                      reads=[dbufs["S"]], writes=[bss_[(tb + 1) % 2]])
            for g0 in range(8):
                pair = (g0, g0 + 8)
                for k_, g in enumerate(pair):
                    K.op(DVE, lambda e, g=g: e.max(out=v16[0:tp, g, 0:8], in_=s_sb[0:tp, g, :]), reads=[bs],
                         writes=[bvg[g]])
                for k_, g in enumerate(pair):
                    K.op(DVE, lambda e, g=g, k_=k_: e.match_replace(out=tmpA2[k_][0:tp, :], in_to_replace=v16[0:tp, g, 0:8],
                                                                    in_values=s_sb[0:tp, g, :], imm_value=NEG),
                         reads=[bs, bvg[g]], writes=[btA2[k_]])
                for k_, g in enumerate(pair):
                    K.op(DVE, lambda e, g=g, k_=k_: e.max(out=v16[0:tp, g, 8:16], in_=tmpA2[k_][0:tp, :]),
                         reads=[btA2[k_]], writes=[bvg[g]])
            vv = v16[0:tp, :, :].rearrange("p (h two) n -> p h two n", two=2)
            K.op(DVE, lambda e: e.tensor_tensor(out=cand[0:tp, :, :].rearrange("p h (i j) -> p h i j", i=16),
                                                in0=vv[:, :, 0, :].unsqueeze(3).to_broadcast([tp, 8, 16, 16]),
                                                in1=vv[:, :, 1, :].unsqueeze(2).to_broadcast([tp, 8, 16, 16]),
                                                op=ALU.add), reads=bvg, writes=[bcand, bv])
            for h0 in range(4):
                pair = (h0, h0 + 4)
                for k_, h in enumerate(pair):
                    K.op(DVE, lambda e, h=h: e.max(out=ctop[0:tp, h, 0:8], in_=cand[0:tp, h, :]), reads=[bcand],
                         writes=[bcth[h]])
                for k_, h in enumerate(pair):
                    K.op(DVE, lambda e, h=h: e.max_index(out=cidx[0:tp, h, 0:8], in_max=ctop[0:tp, h, 0:8],
                                                         in_values=cand[0:tp, h, :]), reads=[bcand, bcth[h]],
                         writes=[bcth[h]])
                for k_, h in enumerate(pair):
                    K.op(DVE, lambda e, h=h, k_=k_: e.match_replace(out=tmpB2[k_][0:tp, :], in_to_replace=ctop[0:tp, h, 0:8],
                                                                    in_values=cand[0:tp, h, :], imm_value=NEG),
                         reads=[bcand, bcth[h]], writes=[btB2[k_]])
                for k_, h in enumerate(pair):
                    K.op(DVE, lambda e, h=h, k_=k_: e.max(out=ctop[0:tp, h, 8:16], in_=tmpB2[k_][0:tp, :]),
                         reads=[btB2[k_]], writes=[bcth[h]])
                for k_, h in enumerate(pair):
                    K.op(DVE, lambda e, h=h, k_=k_: e.max_index(out=cidx[0:tp, h, 8:16], in_max=ctop[0:tp, h, 8:16],
                                                                in_values=tmpB2[k_][0:tp, :]), reads=[btB2[k_], bcth[h]],
                         writes=[bcth[h]])
            K.op(DVE, lambda e: e.tensor_copy(out=joinj[0:tp, 0:1], in_=ctop[0:tp, 0, 0:1]), reads=bcth, writes=[bct])
            K.op(DVE, lambda e: e.tensor_tensor(out=gate[0:tp, :, :], in0=ctop[0:tp, :, :],
                                                in1=ctop[0:tp, :, 0:1].to_broadcast([tp, 8, 16]), op=ALU.subtract),
                 reads=[bct], writes=[bgate])
            K.op(ACT, lambda e: e.activation(out=gate[0:tp, :, :], in_=gate[0:tp, :, :], func=AF.Exp), reads=[bgate],
                 writes=[bgate])
            K.op(DVE, lambda e: e.tensor_reduce(out=zs[0:tp, :], in_=gate[0:tp, :, :], axis=AX.X, op=ALU.add),
                 reads=[bgate], writes=[bgate])
            K.op(DVE, lambda e: e.reciprocal(out=zs[0:tp, :], in_=zs[0:tp, :]), reads=[bgate], writes=[bgate])
            K.op(DVE, lambda e: e.tensor_tensor(out=gate[0:tp, :, :], in0=gate[0:tp, :, :],
                                                in1=zs[0:tp, :].unsqueeze(2).to_broadcast([tp, 8, 16]), op=ALU.mult),
                 reads=[bgate], writes=[bgate])
            K.op(DVE, lambda e: e.tensor_single_scalar(out=iu[0:tp, :, :], in_=cidx[0:tp, :, :], scalar=4,
                                                       op=ALU.logical_shift_right), reads=[bct], writes=[bij])
            K.op(DVE, lambda e: e.tensor_single_scalar(out=ju[0:tp, :, :], in_=cidx[0:tp, :, :], scalar=15,
                                                       op=ALU.bitwise_and), reads=[bct], writes=[bij])
            K.op(DVE, lambda e: e.tensor_copy(out=i_f[0:tp, :, :], in_=iu[0:tp, :, :]), reads=[bij], writes=[bij])
            K.op(DVE, lambda e: e.tensor_copy(out=j_f[0:tp, :, :], in_=ju[0:tp, :, :]), reads=[bij], writes=[bij])
            for w_, pf in enumerate((i_f, j_f)):
                K.op(DVE, lambda e, pf=pf: e.tensor_tensor(
                    out=oh[0:tp], in0=pf[0:tp, :, :].unsqueeze(3).to_broadcast([tp, 8, 16, 16]),
                    in1=iota16[0:tp, :].unsqueeze(1).unsqueeze(1).to_broadcast([tp, 8, 16, 16]), op=ALU.is_equal),
                     reads=[bij, cst], writes=[boh])
                K.op(DVE, lambda e, w_=w_: e.tensor_tensor(
                    out=oh[0:tp], in0=oh[0:tp], in1=vv[:, :, w_, :].unsqueeze(2).to_broadcast([tp, 8, 16, 16]),
                    op=ALU.mult), reads=[boh, bv], writes=[boh])
                K.op(DVE, lambda e, w_=w_: e.tensor_reduce(out=vsel[0:tp, w_, :, :], in_=oh[0:tp], axis=AX.X,
                                                           op=ALU.add), reads=[boh], writes=[bvsel])
            s4 = s_sb[0:tp, :, :].rearrange("p (h two) k -> p h two k", two=2)
            for ih in range(4):
                E1, E2, bE1, bE2 = E1s[ectr % 2], E2s[ectr % 2], bE1s[ectr % 2], bE2s[ectr % 2]
                ectr += 1
                K.op(DVE, lambda e: e.tensor_tensor(
                    out=E1[0:tp], in0=s4[:, :, 0, ih * 32:(ih + 1) * 32].unsqueeze(2).to_broadcast([tp, 8, 16, 32]),
                    in1=vsel[0:tp, 0, :, :].unsqueeze(3).to_broadcast([tp, 8, 16, 32]), op=ALU.is_equal),
                     reads=[bs, bvsel], writes=[bE1])
                K.op(POOL, lambda e: e.tensor_tensor(
                    out=E1[0:tp], in0=E1[0:tp], in1=gate[0:tp, :, :].unsqueeze(3).to_broadcast([tp, 8, 16, 32]),
                    op=ALU.mult), reads=[bE1, bgate], writes=[bE1])
                K.op(DVE, lambda e: e.tensor_tensor(
                    out=E2[0:tp], in0=s4[:, :, 1, ih * 32:(ih + 1) * 32].unsqueeze(2).to_broadcast([tp, 8, 16, 32]),
                    in1=vsel[0:tp, 1, :, :].unsqueeze(3).to_broadcast([tp, 8, 16, 32]), op=ALU.is_equal),
                     reads=[bs, bvsel], writes=[bE2])
                for (Ex, bEx, XT, bXT, pbase) in ((E1, bE1, XT1, bXT1, 0), (E2, bE2, XT2, bXT2, 2)):
                    Ef = Ex[0:tp].rearrange("p h n i -> p (h n) i")
                    for i8 in range(4):
                        pb = pbase + (i8 % 2)
                        pst = psb[pb][:].bitcast(BF16)
                        for ii in range(8):
                            il = i8 * 8 + ii
                            K.op(PE, lambda e, il=il, ii=ii: e.transpose(out=pst[:, ii * 128:ii * 128 + tp],
                                                                         in_=Ef[:, :, il], identity=ident[0:tp, 0:tp]),
                                 reads=[bEx, cst], writes=[pbuf[pb]], signal=(ii == 7))
                        i1_0 = ih * 32 + i8 * 8
                        K.op(ACT, lambda e: e.activation(
                            out=XT[:, 0:tp, i1_0:i1_0 + 8],
                            in_=pst[:, :].rearrange("p (i t) -> p t i", i=8)[:, 0:tp, :], func=AF.Copy),
                             reads=[pbuf[pb]], writes=[bXT])
            for t4 in range(tp // 4):
                pb = 4 + (t4 % 4)
                psg = psb[pb][:, :].rearrange("p (i t) -> p i t", t=4)
                for tt in range(4):
                    t = t4 * 4 + tt
                    K.op(PE, lambda e, t=t, tt=tt: e.matmul(psg[:, :, tt], lhsT=XT1[:, t, :],
                                                            rhs=XT2[:, t, :], start=True, stop=True),
                         reads=[bXT1, bXT2], writes=[pbuf[pb]], signal=(tt == 3))
                K.op(ACT, lambda e: e.activation(out=Gsb[:, :, t4 * 4:t4 * 4 + 4], in_=psg[:, :, :], func=AF.Copy),
                     reads=[pbuf[pb]], writes=[bGsb])
            for q4 in range(4):
                K.dma(POOL,
                      Gs_d[tb, q4 * 32:(q4 + 1) * 32, :, :].rearrange("i2 i1 t -> i1 i2 t"),
                      Gsb[:, q4 * 32:(q4 + 1) * 32, :], reads=[bGsb], writes=[dbufs["Gs"]])
        K.barrier()
    if stop_after <= 5.5:
        K.finish()
        return nc


    eu_v = exp_u.rearrange("(a b) d -> b a d", b=128)
    with ExitStack() as st:
        def sb(name, shape, dt=F32):
            return st.enter_context(nc.sbuf_tensor(name, list(shape), dt))
        h2T = sb("h2Tb", [128, 16, NOWN], BF16)
        bh2T = [Buf() for _ in range(16)]
        for c in range(16):
            K.dma(SP if c % 2 == 0 else POOL, h2T[:, c, :], H2_d[c, :, :], reads=[dbufs["H2"]], writes=[bh2T[c]])
        uf = [sb(f"uf{i}", [128, D]) for i in range(3)]
        ub = [sb(f"ub{i}", [128, D], BF16) for i in range(2)]
        UT = [sb(f"UT{i}", [128, 16, 128], BF16) for i in range(2)]
        gt = [sb(f"gt{i}", [128, NOWN], BF16) for i in range(2)]
        wt = [sb(f"wt{i}", [128, NOWN], BF16) for i in range(2)]
        gl = [sb(f"gl{i}", [128, 512], BF16) for i in range(3)]
        buf_ = [Buf() for _ in range(3)]
        bub = [Buf(), Buf()]
        bUT = [Buf(), Buf()]
        bgt = [Buf(), Buf()]
        bwt = [Buf(), Buf()]
        bgl = [Buf() for _ in range(3)]

        def p6_load(i2):
            K.dma(SP, uf[i2 % 3][:], eu_v[i2], writes=[buf_[i2 % 3]])

        def p6_prep(i2):
            k = i2 % 2
            u3 = i2 % 3
            K.dma(SP, gt[k][:, 0:2048].rearrange("p (b t) -> p b t", b=16),
                  Gs_d[0:16, i2, :, :].rearrange("b i t -> i b t"), reads=[dbufs["Gs"]], writes=[bgt[k]])
            K.dma(SP, gt[k][:, 2048:2112], Gs_d[16, i2, :, 0:64], reads=[dbufs["Gs"]], writes=[bgt[k]])
            K.op(DVE, lambda e: e.tensor_copy(out=ub[k][:], in_=uf[u3][:]), reads=[buf_[u3]], writes=[bub[k]])
            for half in range(2):
                pb = 6 + half
                pst = psb[pb][:].bitcast(BF16)
                for cc in range(8):
                    c = half * 8 + cc
                    K.op(PE, lambda e, c=c, cc=cc: e.transpose(out=pst[:, cc * 128:(cc + 1) * 128],
                                                               in_=ub[k][:, c * 128:(c + 1) * 128], identity=ident[:]),
                         reads=[bub[k], cst], writes=[pbuf[pb]], signal=(cc == 7))
                if half == 0:
                    K.op(ACT, lambda e: e.activation(out=UT[k][:, 0:8, :].rearrange("p c i -> p (c i)"), in_=pst[:, :],
                                                     func=AF.Copy), reads=[pbuf[pb]], writes=[bUT[k]])
                else:
                    K.op(DVE, lambda e: e.tensor_copy(out=UT[k][:, 8:16, :].rearrange("p c i -> p (c i)"), in_=pst[:, :]),
                         reads=[pbuf[pb]], writes=[bUT[k]])

        gi = 0
        p6_load(0)
        p6_load(1)
        p6_prep(0)
        for i2 in range(128):
            k = i2 % 2
            if i2 + 2 < 128:
                p6_load(i2 + 2)
            if i2 + 1 < 128:
                p6_prep(i2 + 1)
            for ti_, (n0, n) in enumerate(own_tiles):
                pb = gi % 6
                g3 = gi % 3
                gi += 1
                for c in range(16):
                    K.op(PE, lambda e, c=c: e.matmul(psb[pb][:, 0:n], lhsT=UT[k][:, c, :], rhs=h2T[:, c, n0:n0 + n],
                                                     start=(c == 0), stop=(c == 15)), reads=[bUT[k], bh2T[c]],
                         writes=[pbuf[pb]], signal=(c == 15))
                K.op(ACT, lambda e: e.activation(out=gl[g3][:, 0:n], in_=psb[pb][:, 0:n], func=AF.Gelu),
                     reads=[pbuf[pb]], writes=[bgl[g3]])
                K.op(POOL, lambda e: e.tensor_tensor(out=wt[k][:, n0:n0 + n], in0=gl[g3][:, 0:n], in1=gt[k][:, n0:n0 + n],
                                                     op=ALU.mult), reads=[bgl[g3], bgt[k]], writes=[bwt[k]])
            K.dma(POOL, Ws_d[i2, :, :], wt[k][:], reads=[bwt[k]], writes=[dbufs["Ws"]])
        K.barrier()
    if stop_after <= 6:
        K.finish()
        return nc

    ev_v = exp_v.rearrange("(a b) d -> b a d", b=128)
    EG = 4
    with ExitStack() as st:
        def sb(name, shape, dt=F32):
            return st.enter_context(nc.sbuf_tensor(name, list(shape), dt))
        acc = sb("acc", [128, 17, 1024])
        bacc = Buf()
        wts = [sb(f"wts{i}", [128, EG, NOWN], BF16) for i in range(2)]
        bwts = [Buf(), Buf()]
        vb = [sb(f"vb{i}", [128, EG, 1024], BF16) for i in range(2)]
        bvb = [Buf(), Buf()]
        vf = [sb(f"vf7{i}", [128, 1024]) for i in range(3)]
        bvf = [Buf() for _ in range(3)]
        G2 = sb("G2", [128, 1024])
        bG2 = Buf()
        x1t = [sb(f"x1t{i}", [128, 1024]) for i in range(2)]
        bx1t = [Buf(), Buf()]
        vi = 0
        for dh in range(2):
            for eg in range(128 // EG):
                k = eg % 2
                K.dma(SP, wts[k][:], Ws_d[eg * EG:(eg + 1) * EG, :, :].rearrange("e p t -> p e t"),
                      reads=[dbufs["Ws"]], writes=[bwts[k]])
                for e_ in range(EG):
                    i2 = eg * EG + e_
                    v3 = vi % 3
                    vi += 1
                    K.dma(SP, vf[v3][:], ev_v[i2, :, dh * 1024:(dh + 1) * 1024], writes=[bvf[v3]])
                    K.op(ACT, lambda e, v3=v3, e_=e_: e.activation(out=vb[k][:, e_, :], in_=vf[v3][:], func=AF.Copy),
                         reads=[bvf[v3]], writes=[bvb[k]])
                for tb, (t0, tp) in enumerate(own_blocks):
                    pbs = (2 * (tb % 4), 2 * (tb % 4) + 1)
                    for e_ in range(EG):
                        for dt_ in range(2):
                            K.op(PE, lambda e, e_=e_, dt_=dt_: e.matmul(psb[pbs[dt_]][0:tp, :], lhsT=wts[k][:, e_, t0:t0 + tp],
                                                                        rhs=vb[k][:, e_, dt_ * 512:(dt_ + 1) * 512],
                                                                        start=(e_ == 0), stop=(e_ == EG - 1)),
                                 reads=[bwts[k], bvb[k]], writes=[pbuf[pbs[dt_]]], signal=(e_ == EG - 1))
                    for dt_ in range(2):
                        dst = acc[0:tp, tb, dt_ * 512:(dt_ + 1) * 512]
                        if eg == 0:
                            K.op(DVE, lambda e, dt_=dt_, dst=dst: e.tensor_copy(out=dst, in_=psb[pbs[dt_]][0:tp, :]),
                                 reads=[pbuf[pbs[dt_]]], writes=[bacc])
                        else:
                            K.op(DVE, lambda e, dt_=dt_, dst=dst: e.tensor_tensor(out=dst, in0=psb[pbs[dt_]][0:tp, :],
                                                                                  in1=dst, op=ALU.add),
                                 reads=[pbuf[pbs[dt_]], bacc], writes=[bacc])
            K.dma(SP, G2[:], gS_d[1, 0, dh * 1024:(dh + 1) * 1024].unsqueeze(0).to_broadcast([128, 1024]),
                  reads=[dbufs["gS"]], writes=[bG2])
            for tb, (t0, tp) in enumerate(own_blocks):
                if tb == 16:
                    for s_ in range(4):
                        K.dma(SP, G2[16 * s_:16 * s_ + 16, :],
                              gS_d[1, 1 + s_, dh * 1024:(dh + 1) * 1024].unsqueeze(0).to_broadcast([16, 1024]),
                              reads=[dbufs["gS"]], writes=[bG2])
                xt, bxt = x1t[tb % 2], bx1t[tb % 2]
                K.dma(SP, xt[0:tp, :], X1_d[t0:t0 + tp, dh * 1024:(dh + 1) * 1024], reads=[dbufs["X1"]], writes=[bxt])
                K.op(DVE, lambda e: e.tensor_tensor(out=acc[0:tp, tb, :], in0=acc[0:tp, tb, :], in1=G2[0:tp, :],
                                                    op=ALU.mult), reads=[bacc, bG2], writes=[bacc])
                K.op(POOL, lambda e: e.tensor_tensor(out=xt[0:tp, :], in0=xt[0:tp, :], in1=acc[0:tp, tb, :], op=ALU.add),
                     reads=[bacc, bxt], writes=[bxt])
                K.dma(POOL, X1_d[t0:t0 + tp, dh * 1024:(dh + 1) * 1024], xt[0:tp, :], reads=[bxt], writes=[dbufs["X1"]])
        K.barrier()
    if stop_after <= 7:
        K.finish()
        return nc

    with ExitStack() as st:
        def sb(name, shape, dt=F32):
            return st.enter_context(nc.sbuf_tensor(name, list(shape), dt))
        FG = sb("FG", [128, D])
        bFG = Buf()
        K.dma(SP, FG[:], final_g.unsqueeze(0).to_broadcast([128, D]), writes=[bFG])
        xf = [sb(f"xf{i}", [128, D]) for i in range(3)]
        bxf = [Buf() for _ in range(3)]
        yf = [sb(f"yf{i}", [128, D]) for i in range(2)]
        byf = [Buf(), Buf()]
        junk = sb("junk8", [128, D], BF16)
        bjunk = Buf()
        ss8 = [sb(f"ss8{i}", [128, 2]) for i in range(2)]
        bss8 = [Buf(), Buf()]
        for tb, (t0, tp) in enumerate(own_blocks):
            xt, bxt = xf[tb % 3], bxf[tb % 3]
            yt, byt = yf[tb % 2], byf[tb % 2]
            ss, bss = ss8[tb % 2], bss8[tb % 2]
            K.dma(SP, xt[0:tp, :], X1_d[t0:t0 + tp, :], reads=[dbufs["X1"]], writes=[bxt])
            K.op(ACT, lambda e: e.activation(out=junk[0:tp, :], in_=xt[0:tp, :], func=AF.Square,
                                             accum_out=ss[0:tp, 0:1]), reads=[bxt], writes=[bss, bjunk])
            K.op(ACT, lambda e: e.activation(out=ss[0:tp, 1:2], in_=ss[0:tp, 0:1], func=AF.Ln, scale=1.0 / D,
                                             bias=eps1[0:tp, :]), reads=[bss, cst], writes=[bss])
            K.op(ACT, lambda e: e.activation(out=ss[0:tp, 1:2], in_=ss[0:tp, 1:2], func=AF.Exp, scale=-0.5),
                 reads=[bss], writes=[bss])
            K.op(DVE, lambda e: e.scalar_tensor_tensor(out=yt[0:tp, :], in0=xt[0:tp, :], scalar=ss[0:tp, 1:2],
                                                       in1=FG[0:tp, :], op0=ALU.mult, op1=ALU.mult),
                 reads=[bxt, bss, bFG], writes=[byt])
            K.dma(POOL, y_out[t0:t0 + tp, :], yt[0:tp, :], reads=[byt], is_output=True)
        K.barrier()

    K.finish()
    return nc


_NC_CACHE = {}


def _get_nc(debug=False, stop_after=99):
    key = (debug, stop_after)
    if key not in _NC_CACHE:
        _NC_CACHE[key] = build(debug=debug, stop_after=stop_after)
    return _NC_CACHE[key]


def make_in_maps(inputs):
    f = lambda k: np.ascontiguousarray(np.asarray(inputs[k], dtype=np.float32))
    xp, xs = f("x_prompt"), f("x_sample")
    cp, cs = f("c_prompt"), f("c_sample")
    ck, cv, stc = f("cache_k")[0], f("cache_v")[0], f("state_conv")[0]
    shared = {
        "norm1_g": f("norm1_g")[0], "norm2_g": f("norm2_g")[0], "final_g": f("final_norm_g"),
        "w_ada": f("w_ada")[0], "b_ada": f("b_ada")[0], "w_in": f("w_in")[0], "conv_w": f("conv_w")[0],
        "w_attn_proj": f("w_attn_proj")[0], "w_conv_out": f("w_conv_out")[0], "w_o": f("w_o")[0],
        "w_query": f("w_query")[0], "sub_keys": f("sub_keys")[0].reshape(16, 128, 128),
        "expert_u": f("expert_u")[0], "expert_v": f("expert_v")[0],
    }
    in_maps = []
    for c in range(8):
        b, half = c // 2, c % 2
        own = xp[b, half * 2048:(half + 1) * 2048]
        ctx = xp[b, 0:2048] if half == 1 else np.zeros((2048, D), np.float32)
        xall = np.concatenate([ctx, own, xs[4 * c:4 * c + 4].reshape(64, D)], axis=0)
        cvec = np.concatenate([cp[b:b + 1], cs[4 * c:4 * c + 4]], axis=0)
        flags = np.zeros((128, 2), np.float32)
        flags[:, 0] = 0.0 if half == 1 else -30000.0
        flags[:, 1] = 1.0 if half == 1 else 0.0
        m = dict(shared)
        m.update({
            "xall": np.ascontiguousarray(xall), "cvec": np.ascontiguousarray(cvec), "flags": flags,
            "cache_k": np.ascontiguousarray(ck[4 * c:4 * c + 4].reshape(4, 4096, 1024)),
            "cache_v": np.ascontiguousarray(cv[4 * c:4 * c + 4].reshape(4, 4096, 1024)),
            "state_conv": np.ascontiguousarray(stc[4 * c:4 * c + 4]),
        })
        in_maps.append(m)
    return in_maps


def kernel(**inputs):
    nc = _get_nc()
    in_maps = make_in_maps(inputs)
    in_maps = [{k: v for k, v in m.items() if k in LAST_INPUT_NAMES} for m in in_maps]
    res = run_bass_kernel_spmd(nc, in_maps, core_ids=list(range(8)))
    R = res.results
    y_prompt = np.zeros((4, 4096, D), np.float32)
    y_sample = np.zeros((32, 16, D), np.float32)
    nkp = np.zeros((1, 4, 4096, 8, 128), np.float32)
    nvp = np.zeros((1, 4, 4096, 8, 128), np.float32)
    ncp = np.zeros((1, 4, 2, 1024), np.float32)
    nks = np.zeros((1, 32, 16, 8, 128), np.float32)
    nvs = np.zeros((1, 32, 16, 8, 128), np.float32)
    ncs = np.zeros((1, 32, 2, 1024), np.float32)
    for c in range(8):
        b, half = c // 2, c % 2
        r = R[c]
        sl = slice(half * 2048, (half + 1) * 2048)
        y_prompt[b, sl] = r["y"][:2048]
        y_sample[4 * c:4 * c + 4] = r["y"][2048:].reshape(4, 16, D)
        nkp[0, b, sl] = r["nk"][:2048].reshape(2048, 8, 128)
        nvp[0, b, sl] = r["nv"][:2048].reshape(2048, 8, 128)
        nks[0, 4 * c:4 * c + 4] = r["nk"][2048:].reshape(4, 16, 8, 128)
        nvs[0, 4 * c:4 * c + 4] = r["nv"][2048:].reshape(4, 16, 8, 128)
        if half == 1:
            ncp[0, b] = r["nconv"][0]
        ncs[0, 4 * c:4 * c + 4] = r["nconv"][1:]
    return (y_prompt, y_sample, nkp, nvp, ncp, nks, nvs, ncs)
```
